# Optimizing a Trainium2 kernel written in Bass

```python
import jax, jax.numpy as jnp
from jax import lax
import numpy as np

D_MODEL = 2048
BATCH = 8
SEQ = 2048
DEPTH = 2

EPS = 1e-6
D_MIX = 2 * D_MODEL
CONV_K = 4
CHUNK = 128
SSD_WIDTH = D_MIX // 2
SSD_HEAD_DIM = 64
SSD_HEADS = SSD_WIDTH // SSD_HEAD_DIM
SSD_GROUPS = 8
SSD_STATE = 128
SSD_XBC = SSD_WIDTH + 2 * SSD_GROUPS * SSD_STATE
LRU_WIDTH = D_MIX // 4
LRU_BLOCKS = 16
LRU_BLOCK = LRU_WIDTH // LRU_BLOCKS
LRU_C = 8.0
MLSTM_WIDTH = D_MIX // 4
MLSTM_HEADS = 4
MLSTM_HEAD_DIM = MLSTM_WIDTH // MLSTM_HEADS
MLSTM_QKV_BLOCK = 4
MLSTM_NBLK = MLSTM_WIDTH // MLSTM_QKV_BLOCK
MLSTM_KSCALE = MLSTM_HEAD_DIM ** -0.5
D_FF = ((8 * D_MODEL + 3 * 256 - 1) // (3 * 256)) * 256
IN_SIZES = (SSD_WIDTH, SSD_XBC, SSD_HEADS, LRU_WIDTH, LRU_WIDTH, MLSTM_WIDTH, MLSTM_WIDTH)
D_IN = sum(IN_SIZES)
IN_SPLITS = [sum(IN_SIZES[:i + 1]) for i in range(len(IN_SIZES) - 1)]

kernel_name = "hymba_ssd_rglru_mlstm_hybrid"


def rmsnorm(x, w):
    xf = x.astype(jnp.float32)
    y = xf * lax.rsqrt(jnp.mean(xf * xf, axis=-1, keepdims=True) + EPS)
    return (y * w).astype(x.dtype)


def causal_conv(x, w, b):
    c = x.shape[-1]
    y = lax.conv_general_dilated(x, w[:, None, :].astype(x.dtype), window_strides=(1,),
                                 padding=[(CONV_K - 1, 0)],
                                 dimension_numbers=('NWC', 'WIO', 'NWC'),
                                 feature_group_count=c)
    return y + b


def ssd_chunked(x, dt, a, bm, cm):
    b, s, h, p = x.shape
    g, n = bm.shape[2], bm.shape[3]
    j = h // g
    c = s // CHUNK
    l = CHUNK
    xd = (x * dt[..., None]).reshape(b, c, l, g, j, p)
    cs = jnp.cumsum((dt * a).reshape(b, c, l, g, j), axis=2)
    bc = bm.reshape(b, c, l, g, n)
    cc = cm.reshape(b, c, l, g, n)
    causal = jnp.tril(jnp.ones((l, l), dtype=bool))[None, None, :, :, None, None]
    seg = cs[:, :, :, None] - cs[:, :, None, :]
    decay = jnp.exp(jnp.where(causal, seg, -jnp.inf))
    scores = jnp.einsum('bctgn,bcsgn->bctsg', cc, bc)
    y_diag = jnp.einsum('bctsgj,bcsgjp->bctgjp', scores[..., None] * decay, xd)
    to_end = jnp.exp(cs[:, :, -1:] - cs)
    states = jnp.einsum('bcsgn,bcsgjp->bcgjpn', bc, xd * to_end[..., None])
    chunk_decay = jnp.exp(cs[:, :, -1])

    def step(h_prev, inp):
        st, dec = inp
        return h_prev * dec[..., None, None] + st, h_prev

    h0 = jnp.zeros((b, g, j, p, n), jnp.float32)
    _, h_in = lax.scan(step, h0, (jnp.moveaxis(states, 1, 0), jnp.moveaxis(chunk_decay, 1, 0)))
    h_in = jnp.moveaxis(h_in, 0, 1)
    y_off = jnp.einsum('bctgn,bcgjpn->bctgjp', cc, h_in) * jnp.exp(cs)[..., None]
    return (y_diag + y_off).reshape(b, s, h, p)


def ssd_mixer(z, xbc, dt_raw, conv_w, conv_b, dt_bias, a_log, d_skip, norm_w):
    b, s, _ = z.shape
    xbc = jax.nn.silu(causal_conv(xbc, conv_w, conv_b).astype(jnp.float32))
    xs, bm, cm = jnp.split(xbc, [SSD_WIDTH, SSD_WIDTH + SSD_GROUPS * SSD_STATE], axis=-1)
    xs = xs.reshape(b, s, SSD_HEADS, SSD_HEAD_DIM)
    bm = bm.reshape(b, s, SSD_GROUPS, SSD_STATE)
    cm = cm.reshape(b, s, SSD_GROUPS, SSD_STATE)
    dt = jax.nn.softplus(dt_raw.astype(jnp.float32) + dt_bias)
    a = -jnp.exp(a_log.astype(jnp.float32))
    y = ssd_chunked(xs, dt, a, bm, cm) + d_skip[:, None] * xs
    y = y.reshape(b, s, SSD_WIDTH) * jax.nn.silu(z.astype(jnp.float32))
    yg = y.reshape(b, s, SSD_GROUPS, SSD_WIDTH // SSD_GROUPS)
    yg = yg * lax.rsqrt(jnp.mean(yg * yg, axis=-1, keepdims=True) + EPS)
    return yg.reshape(b, s, SSD_WIDTH) * norm_w


def rglru_mixer(gate, xr, conv_w, conv_b, w_a, b_a, w_x, b_x, lam):
    b, s, _ = xr.shape
    xc = causal_conv(xr, conv_w, conv_b).astype(jnp.float32)
    xb = xc.reshape(b, s, LRU_BLOCKS, LRU_BLOCK)
    r = jax.nn.sigmoid(jnp.einsum('bsnc,ncd->bsnd', xb, w_a).reshape(b, s, LRU_WIDTH) + b_a)
    i = jax.nn.sigmoid(jnp.einsum('bsnc,ncd->bsnd', xb, w_x).reshape(b, s, LRU_WIDTH) + b_x)
    log_a = -LRU_C * r * jax.nn.softplus(-lam.astype(jnp.float32))
    a = jnp.exp(log_a)
    u = jnp.sqrt(-jnp.expm1(2.0 * log_a)) * (i * xc)

    def combine(e1, e2):
        a1, u1 = e1
        a2, u2 = e2
        return a1 * a2, a2 * u1 + u2

    _, hs = lax.associative_scan(combine, (a, u), axis=1)
    return hs * jax.nn.gelu(gate.astype(jnp.float32), approximate=True)


def mlstm_chunked(q, k, v, i_pre, log_f):
    b, s, h, d = q.shape
    c = s // CHUNK
    l = CHUNK
    qc = q.reshape(b, c, l, h, d)
    kc = k.reshape(b, c, l, h, d)
    vc = v.reshape(b, c, l, h, d)
    ic = i_pre.reshape(b, c, l, h)
    bcum = jnp.cumsum(log_f.reshape(b, c, l, h), axis=2)
    causal = jnp.tril(jnp.ones((l, l), dtype=bool))[None, None, :, :, None]
    dmat = jnp.where(causal, bcum[:, :, :, None] - bcum[:, :, None, :] + ic[:, :, None, :], -jnp.inf)
    w_end = bcum[:, :, -1:] - bcum + ic
    m_loc = jnp.max(w_end, axis=2)
    p_end = jnp.exp(w_end - m_loc[:, :, None])
    c_loc = jnp.einsum('bcshd,bcshe->bchde', vc * p_end[..., None], kc)
    n_loc = jnp.einsum('bcsh,bcshe->bche', p_end, kc)
    g_tot = bcum[:, :, -1]

    def step(carry, inp):
        cm, nm, mm = carry
        cl, nl, ml, gl = inp
        m_new = jnp.maximum(gl + mm, ml)
        s_old = jnp.exp(gl + mm - m_new)
        s_loc = jnp.exp(ml - m_new)
        c_new = s_old[..., None, None] * cm + s_loc[..., None, None] * cl
        n_new = s_old[..., None] * nm + s_loc[..., None] * nl
        return (c_new, n_new, m_new), (cm, nm, mm)

    init = (jnp.zeros((b, h, d, d), jnp.float32), jnp.zeros((b, h, d), jnp.float32),
            jnp.zeros((b, h), jnp.float32))
    xs = (jnp.moveaxis(c_loc, 1, 0), jnp.moveaxis(n_loc, 1, 0), jnp.moveaxis(m_loc, 1, 0),
          jnp.moveaxis(g_tot, 1, 0))
    _, (c_in, n_in, m_in) = lax.scan(step, init, xs)
    c_in = jnp.moveaxis(c_in, 0, 1)
    n_in = jnp.moveaxis(n_in, 0, 1)
    m_in = jnp.moveaxis(m_in, 0, 1)
    inter_log = bcum + m_in[:, :, None]
    m_t = jnp.maximum(jnp.max(dmat, axis=3), inter_log)
    p_intra = jnp.exp(dmat - m_t[:, :, :, None])
    s_inter = jnp.exp(inter_log - m_t)
    qk = jnp.einsum('bcthd,bcshd->bctsh', qc, kc) * p_intra
    num = (jnp.einsum('bctsh,bcshd->bcthd', qk, vc)
           + s_inter[..., None] * jnp.einsum('bchde,bcthe->bcthd', c_in, qc))
    den = jnp.sum(qk, axis=3) + s_inter * jnp.einsum('bche,bcthe->bcth', n_in, qc)
    out = num / jnp.maximum(jnp.abs(den), jnp.exp(-m_t))[..., None]
    return out.reshape(b, s, h, d)


def mlstm_mixer(mx, o_pre, conv_w, conv_b, w_q, w_k, w_v, w_if, b_if, norm_w):
    b, s, _ = mx.shape
    mxf = mx.astype(jnp.float32)
    mc = jax.nn.silu(causal_conv(mx, conv_w, conv_b).astype(jnp.float32))

    def blockdiag(t, w):
        tb = t.reshape(b, s, MLSTM_NBLK, MLSTM_QKV_BLOCK)
        return jnp.einsum('bsnc,ncd->bsnd', tb, w).reshape(b, s, MLSTM_WIDTH)

    q = blockdiag(mc, w_q)
    k = blockdiag(mc, w_k)
    v = blockdiag(mxf, w_v)
    gates = jnp.concatenate([q, k, v], axis=-1) @ w_if + b_if
    i_pre, f_pre = jnp.split(gates, 2, axis=-1)
    log_f = jax.nn.log_sigmoid(f_pre)
    shp = (b, s, MLSTM_HEADS, MLSTM_HEAD_DIM)
    hh = mlstm_chunked(q.reshape(shp), k.reshape(shp) * MLSTM_KSCALE, v.reshape(shp), i_pre, log_f)
    mu = jnp.mean(hh, axis=-1, keepdims=True)
    var = jnp.mean(jnp.square(hh - mu), axis=-1, keepdims=True)
    hn = ((hh - mu) * lax.rsqrt(var + EPS)).reshape(b, s, MLSTM_WIDTH) * norm_w
    return jax.nn.sigmoid(o_pre.astype(jnp.float32)) * hn


def setup_inputs(seed: int = 0) -> dict:
    key = jax.random.key(seed)
    ks = iter(jax.random.split(key, 40))
    L = DEPTH

    def nrm(shape, scale):
        return jax.random.normal(next(ks), shape, jnp.float32) * scale

    def gain(shape):
        return 1.0 + nrm(shape, 0.02)

    x = nrm((BATCH, SEQ, D_MODEL), 1.0)
    norm1_w = gain((L, D_MODEL))
    w_in = nrm((L, D_MODEL, D_IN), D_MODEL ** -0.5)
    ssd_conv_w = nrm((L, CONV_K, SSD_XBC), 0.5)
    ssd_conv_b = nrm((L, SSD_XBC), 0.02)
    u_dt = jax.random.uniform(next(ks), (L, SSD_HEADS), jnp.float32)
    dt0 = jnp.exp(u_dt * (np.log(0.1) - np.log(0.001)) + np.log(0.001))
    ssd_dt_bias = dt0 + jnp.log(-jnp.expm1(-dt0))
    ssd_a_log = jnp.log(jax.random.uniform(next(ks), (L, SSD_HEADS), jnp.float32, 1.0, 16.0))
    ssd_d = gain((L, SSD_HEADS))
    ssd_norm_w = gain((L, SSD_WIDTH))
    lru_conv_w = nrm((L, CONV_K, LRU_WIDTH), 0.5)
    lru_conv_b = nrm((L, LRU_WIDTH), 0.02)
    lru_w_a = nrm((L, LRU_BLOCKS, LRU_BLOCK, LRU_BLOCK), LRU_BLOCK ** -0.5)
    lru_b_a = nrm((L, LRU_WIDTH), 0.02)
    lru_w_x = nrm((L, LRU_BLOCKS, LRU_BLOCK, LRU_BLOCK), LRU_BLOCK ** -0.5)
    lru_b_x = nrm((L, LRU_WIDTH), 0.02)
    a0 = jax.random.uniform(next(ks), (L, LRU_WIDTH), jnp.float32, 0.9, 0.999)
    sig = a0 ** (1.0 / LRU_C)
    lru_lambda = jnp.log(sig) - jnp.log1p(-sig)
    ml_conv_w = nrm((L, CONV_K, MLSTM_WIDTH), 0.5)
    ml_conv_b = nrm((L, MLSTM_WIDTH), 0.02)
    ml_w_q = nrm((L, MLSTM_NBLK, MLSTM_QKV_BLOCK, MLSTM_QKV_BLOCK), MLSTM_QKV_BLOCK ** -0.5)
    ml_w_k = nrm((L, MLSTM_NBLK, MLSTM_QKV_BLOCK, MLSTM_QKV_BLOCK), MLSTM_QKV_BLOCK ** -0.5)
    ml_w_v = nrm((L, MLSTM_NBLK, MLSTM_QKV_BLOCK, MLSTM_QKV_BLOCK), MLSTM_QKV_BLOCK ** -0.5)
    ml_w_if = nrm((L, 3 * MLSTM_WIDTH, 2 * MLSTM_HEADS), 0.02)
    b_i = nrm((L, MLSTM_HEADS), 0.1)
    b_f = jnp.linspace(3.0, 6.0, MLSTM_HEADS, dtype=jnp.float32)[None, :] + nrm((L, MLSTM_HEADS), 0.1)
    ml_b_if = jnp.concatenate([b_i, b_f], axis=-1)
    ml_norm_w = gain((L, MLSTM_WIDTH))
    w_out = nrm((L, D_MIX, D_MODEL), D_MIX ** -0.5)
    norm2_w = gain((L, D_MODEL))
    w_gate_up = nrm((L, D_MODEL, 2 * D_FF), D_MODEL ** -0.5)
    w_down = nrm((L, D_FF, D_MODEL), D_FF ** -0.5)
    norm_f_w = gain((D_MODEL,))
    return {"x": x, "norm1_w": norm1_w, "w_in": w_in,
            "ssd_conv_w": ssd_conv_w, "ssd_conv_b": ssd_conv_b, "ssd_dt_bias": ssd_dt_bias,
            "ssd_a_log": ssd_a_log, "ssd_d": ssd_d, "ssd_norm_w": ssd_norm_w,
            "lru_conv_w": lru_conv_w, "lru_conv_b": lru_conv_b, "lru_w_a": lru_w_a,
            "lru_b_a": lru_b_a, "lru_w_x": lru_w_x, "lru_b_x": lru_b_x, "lru_lambda": lru_lambda,
            "ml_conv_w": ml_conv_w, "ml_conv_b": ml_conv_b, "ml_w_q": ml_w_q, "ml_w_k": ml_w_k,
            "ml_w_v": ml_w_v, "ml_w_if": ml_w_if, "ml_b_if": ml_b_if, "ml_norm_w": ml_norm_w,
            "w_out": w_out, "norm2_w": norm2_w, "w_gate_up": w_gate_up, "w_down": w_down,
            "norm_f_w": norm_f_w}


def reference(x, norm1_w, w_in, ssd_conv_w, ssd_conv_b, ssd_dt_bias, ssd_a_log, ssd_d, ssd_norm_w,
              lru_conv_w, lru_conv_b, lru_w_a, lru_b_a, lru_w_x, lru_b_x, lru_lambda,
              ml_conv_w, ml_conv_b, ml_w_q, ml_w_k, ml_w_v, ml_w_if, ml_b_if, ml_norm_w,
              w_out, norm2_w, w_gate_up, w_down, norm_f_w):
    h = x
    for l in range(DEPTH):
        u = rmsnorm(h, norm1_w[l])
        proj = jnp.einsum('bsd,de->bse', u, w_in[l])
        z, xbc, dt_raw, lru_gate, lru_x, ml_x, ml_o = jnp.split(proj, IN_SPLITS, axis=-1)
        y_ssd = ssd_mixer(z, xbc, dt_raw, ssd_conv_w[l], ssd_conv_b[l], ssd_dt_bias[l],
                          ssd_a_log[l], ssd_d[l], ssd_norm_w[l])
        y_lru = rglru_mixer(lru_gate, lru_x, lru_conv_w[l], lru_conv_b[l], lru_w_a[l], lru_b_a[l],
                            lru_w_x[l], lru_b_x[l], lru_lambda[l])
        y_ml = mlstm_mixer(ml_x, ml_o, ml_conv_w[l], ml_conv_b[l], ml_w_q[l], ml_w_k[l], ml_w_v[l],
                           ml_w_if[l], ml_b_if[l], ml_norm_w[l])
        y = jnp.concatenate([y_ssd, y_lru, y_ml], axis=-1).astype(h.dtype)
        h = h + jnp.einsum('bse,ed->bsd', y, w_out[l])
        u = rmsnorm(h, norm2_w[l])
        gt, up = jnp.split(jnp.einsum('bsd,df->bsf', u, w_gate_up[l]), 2, axis=-1)
        h = h + jnp.einsum('bsf,fd->bsd', jax.nn.silu(gt) * up, w_down[l])
    return rmsnorm(h, norm_f_w)
```

```python
import contextlib
import numpy as np
import concourse.bass as bass
import concourse.mybir as mybir
from concourse.bass_utils import run_bass_kernel_spmd

F32 = mybir.dt.float32
BF16 = mybir.dt.bfloat16
AF = mybir.ActivationFunctionType
ALU = mybir.AluOpType
AX = mybir.AxisListType

T = 2048
D = 2048
DIN = 10272
DFF = 5632
NL = 2
EPS = 1e-6
NEG = -30000.0
import os
LSTOP = int(os.environ.get('LSTOP', '0'))
SSTOP = float(os.environ.get('SSTOP', '0'))
ENGS = ['pe', 'act', 'dve', 'pool', 'sp']


class Buf:
    __slots__ = ('name', 'lw', 'rd', 'const', 'excl')

    def __init__(self, name, const=False, excl=False):
        self.name = name
        self.lw = None
        self.rd = []
        self.const = const
        self.excl = excl


class Op:
    __slots__ = ('eng', 'fn', 'deps', 'sig', 'dma', 'slot', 'sem', 'val', 'n')


class Sched:
    def __init__(self, nc, ndma=8):
        self.nc = nc
        self.ops = {e: [] for e in ENGS}
        self.ndma = ndma
        self.dma_cnt = {e: 0 for e in ENGS}
        self.dma_last = {}
        self.nops = 0

    def add(self, eng, fn, rd=(), wr=(), dma=False):
        op = Op()
        op.eng = eng
        op.fn = fn
        op.dma = dma
        op.sig = False
        op.n = self.nops
        self.nops += 1
        ex = [b for b in rd if b.excl]
        if ex:
            wr = list(wr) + [b for b in ex if all(b is not w for w in wr)]
        deps = {}
        for b in rd:
            if b.lw is not None:
                deps[id(b.lw)] = b.lw
        for b in wr:
            if b.lw is not None:
                deps[id(b.lw)] = b.lw
            for r in b.rd:
                deps[id(r)] = r
        if dma:
            slot = self.dma_cnt[eng] % self.ndma
            self.dma_cnt[eng] += 1
            op.slot = slot
            prev = self.dma_last.get((eng, slot))
            if prev is not None:
                deps[id(prev)] = prev
            self.dma_last[(eng, slot)] = op
        dl = []
        for d in deps.values():
            if eng == 'pe' and d.eng == 'pe' and not d.dma and not dma:
                continue
            dl.append(d)
        op.deps = dl
        wrs = set(id(b) for b in wr)
        for b in rd:
            if not b.const and id(b) not in wrs:
                b.rd.append(op)
        for b in wr:
            b.lw = op
            b.rd = []
        self.ops[eng].append(op)
        return op

    def barrier(self):
        lasts = [self.ops[e][-1] for e in ENGS if self.ops[e]] + list(self.dma_last.values())
        for e in ENGS:
            op = Op()
            op.eng = e; op.fn = None; op.dma = False; op.sig = False; op.n = self.nops
            op.deps = [d for d in lasts if d.fn is not None]
            self.ops[e].append(op)

    def emit(self):
        nc = self.nc
        fin = Op()
        fin.eng = 'sp'; fin.fn = None; fin.dma = False; fin.sig = False
        fin.deps = list(self.dma_last.values()); fin.n = self.nops
        self.ops['sp'].append(fin)
        for e in ENGS:
            for op in self.ops[e]:
                for d in op.deps:
                    d.sig = True
        with contextlib.ExitStack() as st:
            csem = {e: st.enter_context(nc.semaphore('c_' + e)) for e in ENGS}
            dsem = {}
            for e in ENGS:
                for j in range(min(self.ndma, self.dma_cnt[e])):
                    dsem[(e, j)] = st.enter_context(nc.semaphore('d_%s%d' % (e, j)))
            for e in ENGS:
                cnt = 0
                dcnt = {}
                for op in self.ops[e]:
                    if op.dma:
                        dcnt[op.slot] = dcnt.get(op.slot, 0) + 1
                        op.sem = dsem[(e, op.slot)]
                        op.val = 16 * dcnt[op.slot]
                    elif op.sig:
                        cnt += 1
                        op.sem = csem[e]
                        op.val = cnt
            block = st.enter_context(nc.Block())
            stats = {}

            def run(e, eng):
                waited = {}
                nw = 0
                for op in self.ops[e]:
                    need = {}
                    for d in op.deps:
                        k = id(d.sem)
                        if waited.get(k, 0) >= d.val:
                            continue
                        if k not in need or need[k][1] < d.val:
                            need[k] = (d.sem, d.val)
                    for k, (sem, val) in need.items():
                        eng.wait_ge(sem, val)
                        waited[k] = val
                        nw += 1
                    if op.fn is None:
                        continue
                    ins = op.fn(eng)
                    if op.dma:
                        ins.then_inc(op.sem, 16)
                    elif op.sig:
                        ins.then_inc(op.sem, 1)
                stats[e] = (len(self.ops[e]), nw)

            @block.tensor
            def _(eng):
                run('pe', eng)

            @block.scalar
            def _(eng):
                run('act', eng)

            @block.vector
            def _(eng):
                run('dve', eng)

            @block.gpsimd
            def _(eng):
                run('pool', eng)

            @block.sync
            def _(eng):
                run('sp', eng)
            self.stats = stats


def _cvmap():
    m = {}
    c = 0
    for name, n in [('n1', 16), ('n2', 16), ('scw', 128), ('scb', 32), ('sd', 16), ('snw', 16),
                    ('lcw', 32), ('lcb', 8), ('lba', 8), ('lbx', 8), ('lam', 8),
                    ('mcw', 32), ('mcb', 8), ('mnw', 8), ('nf', 16)]:
        m[name] = c
        c += n
    return m, c


CVM, NCV = _cvmap()


class Ctx:
    pass


def build(nl=NL, dbg=None, mixers=('ssd', 'lru', 'ml'), do_ffn=True, mixtest=False):
    nc = bass.Bass("TRN2", target_bir_lowering=False)
    dbg = dbg or ()
    IN = lambda n, s, dt=F32: nc.dram_tensor(n, list(s), dt, kind="ExternalInput").ap()

    def SCR(n, s, dt=F32):
        kind = "ExternalOutput" if n in dbg else "Internal"
        if mixtest and n in ('zT', 'xbcT', 'dtT', 'lgT', 'lxT', 'mxT', 'moT'):
            kind = "ExternalInput"
        return nc.dram_tensor(n, list(s), dt, kind=kind).ap()
    x = IN("x", [T, D])
    if mixtest:
        w_in = w_out = w_gu = w_dn = None
    else:
        w_in = IN("w_in", [NL, D, DIN]); w_out = IN("w_out", [NL, 2 * D, D])
        w_gu = IN("w_gate_up", [NL, D, 2 * DFF]); w_dn = IN("w_down", [NL, DFF, D])
    cv_d = IN("cv", [NL, 128, NCV])
    hv_d = IN("hv", [NL, 32, 4])
    bdq_d = IN("bdq", [NL, 3, 8, 128, 128])
    lbd_d = IN("lbd", [NL, 2, 8, 128, 128])
    wif_d = IN("wif", [NL, 128, 24, 8])
    cst_d = IN("cst", [128, 128 + 512])
    c32_d = IN("c32", [128, 2048 + 4 + 8 * 128 + 16 * 128])
    nfb_d = IN("nfb", [1, D])
    out_d = nc.dram_tensor("out", [T, D], F32, kind="ExternalOutput").ap()
    hT = SCR("hT", [D, T])
    zT = SCR("zT", [2048, T]); xbcT = SCR("xbcT", [4096, T], BF16); dtT = SCR("dtT", [32, T])
    lgT = SCR("lgT", [1024, T]); lxT = SCR("lxT", [1024, T], BF16)
    mxT = SCR("mxT", [1024, T], BF16); moT = SCR("moT", [1024, T])
    yT = SCR("yT", [4096, T], BF16)
    mq_d = SCR("mqT", [1024, T], BF16); mk_d = SCR("mkT", [1024, T], BF16); mv_d = SCR("mvT", [1024, T], BF16)

    S = Sched(nc)
    with contextlib.ExitStack() as st:
        _tn = [0]

        def TL(shape, dt, name=None):
            _tn[0] += 1
            return st.enter_context(nc.sbuf_tensor(name or ("t%d" % _tn[0]), list(shape), dt))
        identf = TL([128, 128], F32); identb = TL([128, 128], BF16); mneg = TL([128, 512], BF16)
        onesf = TL([128, 128], F32)
        cvs = [TL([128, NCV], F32) for _ in range(NL)]
        B_const = Buf('const', const=True)
        AR = TL([128, 30720], F32, "arena")
        WS = [TL([128, 44 * 128], BF16) for _ in range(3)]
        WSB = [Buf('ws%d' % i) for i in range(3)]
        EV = [TL([128, 2048], F32) for _ in range(2)]
        EVB = [Buf('ev%d' % i) for i in range(2)]
        SM = [TL([128, 512], F32) for _ in range(6)]
        SMB = [Buf('sm%d' % i) for i in range(6)]
        RS = [TL([128, 512], F32) for _ in range(2)]
        RSB = [Buf('rs%d' % i) for i in range(2)]
        PS = [st.enter_context(nc.psum_tensor("ps%d" % i, [128, 512], F32)) for i in range(8)]
        PSB = [Buf('psb%d' % i, excl=True) for i in range(8)]
        B_h = Buf('hT'); B_proj = Buf('proj'); B_y = Buf('yT'); B_out = Buf('out')
        B_AR = Buf('arena')
        B_mq = Buf('mqkv')

        def arv(off, shape, dt):
            n = int(np.prod(shape[1:]))
            nb = n * (2 if dt == BF16 else 4)
            assert off % 4 == 0 and off + nb <= 122880, (off, nb)
            v = AR[0:shape[0], off // 4:(off + nb + 3) // 4]
            if dt == BF16:
                v = v.bitcast(BF16)
            if len(shape) == 3:
                v = v.rearrange("p (a b) -> p a b", b=shape[2])
            elif len(shape) == 4:
                v = v.rearrange("p (a b c) -> p a b c", b=shape[2], c=shape[3])
            return v

        def dma(eng, out, in_, rd, wr):
            return S.add(eng, lambda e: e.dma_start(out=out, in_=in_), rd, wr, dma=True)

        def mm(out, lhsT, rhs, start, stop, rd, wr):
            return S.add('pe', lambda e: e.matmul(out, lhsT=lhsT, rhs=rhs, start=start, stop=stop), rd, wr)

        def tr(out, in_, ident, rd, wr):
            return S.add('pe', lambda e: e.transpose(out, in_, ident), rd, wr)

        def act(out, in_, func, rd, wr, bias=None, scale=None):
            kw = {}
            if bias is not None:
                kw['bias'] = bias
            if scale is not None:
                kw['scale'] = scale
            return S.add('act', lambda e: e.activation(out=out, in_=in_, func=func, **kw), rd, wr)

        def tt(eng, out, a, b, op, rd, wr):
            return S.add(eng, lambda e: e.tensor_tensor(out=out, in0=a, in1=b, op=op), rd, wr)

        def ts(eng, out, a, s1, op0, rd, wr, s2=None, op1=None):
            if op1 is None:
                return S.add(eng, lambda e: e.tensor_scalar(out=out, in0=a, scalar1=s1, scalar2=None, op0=op0), rd, wr)
            return S.add(eng, lambda e: e.tensor_scalar(out=out, in0=a, scalar1=s1, scalar2=s2, op0=op0, op1=op1), rd, wr)

        def stt(out, in0, scalar, in1, op0, op1, rd, wr):
            return S.add('dve', lambda e: e.scalar_tensor_tensor(out=out, in0=in0, scalar=scalar, in1=in1, op0=op0, op1=op1), rd, wr)

        def cp(eng, out, in_, rd, wr):
            if eng == 'act':
                return S.add('act', lambda e: e.activation(out=out, in_=in_, func=AF.Copy), rd, wr)
            return S.add(eng, lambda e: e.tensor_copy(out=out, in_=in_), rd, wr)

        def memset(eng, ap, val, wr):
            return S.add(eng, lambda e: e.memset(ap, val), (), wr)

        def recip(out, in_, rd, wr):
            return S.add('dve', lambda e: e.reciprocal(out=out, in_=in_), rd, wr)

        rot = {}

        def nxt(key, n):
            rot[key] = (rot.get(key, -1) + 1) % n
            return rot[key]

        B_c = Buf('cinit')
        dma('sp', identf[:], cst_d[:, 0:128], [], [B_c])
        dma('pool', identb[:], cst_d[:, 0:128], [], [B_c])
        dma('pool', mneg[:], cst_d[:, 128:640], [], [B_c])
        for l in range(NL):
            dma('sp', cvs[l][:], cv_d[l], [], [B_c])
        memset('dve', onesf[:], 1.0, [B_c])
        S.barrier()

        def stage0():
            for tt_ in range(16):
                s = nxt('ev', 2)
                xt = EV[s]
                dma('sp', xt[:], x[tt_ * 128:(tt_ + 1) * 128, :], [], [EVB[s]])
                s2 = nxt('ev', 2)
                ho = EV[s2][:].rearrange("p (k t) -> p k t", t=128)
                for dg in range(4):
                    b = nxt('ps', 8)
                    for j in range(4):
                        k = dg * 4 + j
                        tr(PS[b][:, j * 128:(j + 1) * 128], xt[:, k * 128:(k + 1) * 128], identf[:], [EVB[s]], [PSB[b]])
                    cp('act' if dg % 2 == 0 else 'dve', EV[s2][:, dg * 512:(dg + 1) * 512], PS[b][:], [PSB[b]], [EVB[s2]])
                dma('sp', hT[:, tt_ * 128:(tt_ + 1) * 128].rearrange("(k p) t -> p k t", p=128), ho, [EVB[s2]], [B_h])

        def norm_stage(cvt, ncol, uT, B_u, tok0, ntok):
            for ti in range(ntok // 512):
                t0 = tok0 + ti * 512
                pb = nxt('ps', 8)
                for k in range(16):
                    s = nxt('sm', 6)
                    dma('sp', SM[s][:], hT[k * 128:(k + 1) * 128, t0:t0 + 512], [B_h], [SMB[s]])
                    s2 = nxt('sm', 6)
                    act(SM[s2][:], SM[s][:], AF.Square, [SMB[s]], [SMB[s2]])
                    mm(PS[pb][:], onesf[:], SM[s2][:], k == 0, k == 15, [SMB[s2]], [PSB[pb]])
                sr = nxt('rs', 2)
                act(RS[sr][:], PS[pb][:], AF.Sqrt, [PSB[pb]], [RSB[sr]], bias=EPS, scale=1.0 / D)
                recip(RS[sr][:], RS[sr][:], [RSB[sr]], [RSB[sr]])
                for k in range(16):
                    s = nxt('sm', 6)
                    dma('sp', SM[s][:], hT[k * 128:(k + 1) * 128, t0:t0 + 512], [B_h], [SMB[s]])
                    stt(uT[:, k, ti * 512:(ti + 1) * 512], SM[s][:], cvt[:, ncol + k:ncol + k + 1], RS[sr][:],
                        ALU.mult, ALU.mult, [SMB[s], RSB[sr]], [B_u])

        def load_w(src, nk, width):
            s = nxt('ws', 3)
            wv = WS[s][:, 0:nk * width].rearrange("p (k m) -> p k m", m=width)
            dma('pool', wv, src.rearrange("(k p) m -> p k m", p=128), [], [WSB[s]])
            return wv, WSB[s]

        def proj_stage(l, uT, B_u):
            segs = [(0, 2048, zT, F32), (2048, 4096, xbcT, BF16), (6144, 32, dtT, F32), (6176, 1024, lgT, F32),
                    (7200, 1024, lxT, BF16), (8224, 1024, mxT, BF16), (9248, 1024, moT, F32)]
            ecnt = 0
            for (c0, n, dst, dt_) in segs:
                wwid = 256 if n >= 256 else n
                for wb in range(n // wwid):
                    wv, wB = load_w(w_in[l][:, c0 + wb * wwid:c0 + (wb + 1) * wwid], 16, wwid)
                    for sb in range(max(1, wwid // 128)):
                        m = min(128, wwid)
                        es = nxt('ev', 2)
                        if dt_ == BF16:
                            evv = EV[es][:].bitcast(BF16)[:, 0:2048]
                        else:
                            evv = EV[es][:]
                        for ti in range(4):
                            pb = nxt('ps', 4)
                            for k in range(16):
                                mm(PS[pb][0:m, :], wv[:, k, sb * 128:sb * 128 + m], uT[:, k, ti * 512:(ti + 1) * 512],
                                   k == 0, k == 15, [wB, B_u], [PSB[pb]])
                            ecnt += 1
                            cp('act' if ecnt % 2 else 'dve', evv[0:m, ti * 512:(ti + 1) * 512], PS[pb][0:m, :], [PSB[pb]], [EVB[es]])
                        r0 = wb * wwid + sb * 128
                        dma('sp', dst[r0:r0 + m, :], evv[0:m, :], [EVB[es]], [B_proj])

        def resid_dense(wsrc, nk, rhsT, B_rhs, tok0, ntok):
            for db in range(16):
                wv, wB = load_w(wsrc[:, db * 128:(db + 1) * 128], nk, 128)
                for ti in range(ntok // 512):
                    t0 = tok0 + ti * 512
                    pb = nxt('ps', 4)
                    s = nxt('sm', 6)
                    dma('sp', SM[s][:], hT[db * 128:(db + 1) * 128, t0:t0 + 512], [B_h], [SMB[s]])
                    for k in range(nk):
                        mm(PS[pb][:], wv[:, k, :], rhsT[:, k, ti * 512:(ti + 1) * 512], k == 0, k == nk - 1,
                           [wB, B_rhs], [PSB[pb]])
                    tt('dve', SM[s][:], SM[s][:], PS[pb][:], ALU.add, [SMB[s], PSB[pb]], [SMB[s]])
                    dma('act', hT[db * 128:(db + 1) * 128, t0:t0 + 512], SM[s][:], [SMB[s]], [B_h])

        def wout_stage(l):
            for half in range(2):
                yv = arv(0, [128, 32, 1024], BF16)
                B_yv = Buf('yv')
                for q in range(4):
                    dma('sp', yv[:, q * 8:(q + 1) * 8, :],
                        yT[q * 1024:(q + 1) * 1024, half * 1024:(half + 1) * 1024].rearrange("(k p) t -> p k t", p=128),
                        [B_y], [B_yv])
                resid_dense(w_out[l], 32, yv, B_yv, half * 1024, 1024)
                S.barrier()

        def ffn_stage(l):
            for half in range(2):
                u2 = arv(0, [128, 16, 1024], BF16)
                aT = arv(32768, [128, 44, 1024], BF16)
                B_u2 = Buf('u2'); B_a = Buf('aT')
                norm_stage(cvs[l], CVM['n2'], u2, B_u2, half * 1024, 1024)
                for fb in range(44):
                    s = nxt('ws', 3)
                    wv = WS[s][:, 0:16 * 256].rearrange("p (k m) -> p k m", m=256)
                    dma('pool', wv[:, :, 0:128], w_gu[l][:, fb * 128:(fb + 1) * 128].rearrange("(k p) m -> p k m", p=128), [], [WSB[s]])
                    dma('pool', wv[:, :, 128:256], w_gu[l][:, DFF + fb * 128:DFF + (fb + 1) * 128].rearrange("(k p) m -> p k m", p=128), [], [WSB[s]])
                    for ti in range(2):
                        pg = nxt('ps', 4)
                        for k in range(16):
                            mm(PS[pg][:], wv[:, k, 0:128], u2[:, k, ti * 512:(ti + 1) * 512], k == 0, k == 15, [WSB[s], B_u2], [PSB[pg]])
                        pu = nxt('ps', 4)
                        for k in range(16):
                            mm(PS[pu][:], wv[:, k, 128:256], u2[:, k, ti * 512:(ti + 1) * 512], k == 0, k == 15, [WSB[s], B_u2], [PSB[pu]])
                        sg = nxt('sm', 6)
                        act(SM[sg][:], PS[pg][:], AF.Silu, [PSB[pg]], [SMB[sg]])
                        tt('dve', aT[:, fb, ti * 512:(ti + 1) * 512], SM[sg][:], PS[pu][:], ALU.mult, [SMB[sg], PSB[pu]], [B_a])
                resid_dense(w_dn[l], 44, aT, B_a, half * 1024, 1024)
                S.barrier()

        def final_stage():
            wB_ = arv(0, [128, 2048], F32)
            B_w = Buf('nfw')
            dma('sp', wB_, nfb_d[0:1, :].partition_broadcast(128), [], [B_w])
            ssq = arv(8192, [128, 16], F32)
            B_s = Buf('ssq')
            hins = [arv(16384 + i * 8192, [128, 16, 128], F32) for i in range(2)]
            htoks = [arv(32768 + i * 8192, [128, 2048], F32) for i in range(2)]
            junk = arv(49152, [128, 2048], F32)
            B_hin = [Buf('hin%d' % i) for i in range(2)]
            B_ht = [Buf('ht%d' % i) for i in range(2)]
            B_j = Buf('junk')
            for tt_ in range(16):
                s = tt_ % 2
                hin = hins[s]
                ht = htoks[s]
                dma('sp', hin, hT[:, tt_ * 128:(tt_ + 1) * 128].rearrange("(k p) t -> p k t", p=128), [B_h], [B_hin[s]])
                for dg in range(4):
                    b = nxt('ps', 8)
                    for j in range(4):
                        k = dg * 4 + j
                        tr(PS[b][:, j * 128:(j + 1) * 128], hin[:, k, :], identf[:], [B_hin[s]], [PSB[b]])
                    cp('act' if dg % 2 == 0 else 'dve', ht[:, dg * 512:(dg + 1) * 512], PS[b][:], [PSB[b]], [B_ht[s]])
                sq = ssq[:, tt_:tt_ + 1]
                S.add('act', (lambda o, i, a_: (lambda e: e.activation(out=o, in_=i, func=AF.Square, accum_out=a_)))(junk, ht, sq),
                      [B_ht[s]], [B_j, B_s])
                act(sq, sq, AF.Sqrt, [B_s], [B_s], bias=EPS, scale=1.0 / D)
                recip(sq, sq, [B_s], [B_s])
                stt(ht, ht, sq, wB_, ALU.mult, ALU.mult, [B_ht[s], B_s, B_w], [B_ht[s]])
                dma('sp', out_d[tt_ * 128:(tt_ + 1) * 128, :], ht, [B_ht[s]], [B_out])

        def mixer_stage(l):
            if not mixers:
                zt = arv(0, [128, 2048], BF16)
                B_z = Buf('zt')
                memset('dve', zt, 0.0, [B_z])
                for q in range(32):
                    dma('sp', yT[q * 128:(q + 1) * 128, :], zt, [B_z], [B_y])
                return
            cvt = cvs[l]
            aoff = [0]

            def NT(shape, dt, name):
                n = int(np.prod(shape[1:])) * (2 if dt == BF16 else 4)
                n = (n + 31) // 32 * 32
                v = arv(aoff[0], shape, dt)
                aoff[0] += n
                return v, Buf(name)

            def areset(o=0):
                aoff[0] = o
            c32, B_c32 = NT([128, 5124], F32, 'c32')
            hvt, B_hv = NT([32, 4], F32, 'hvt')
            dma('sp', c32, c32_d, [], [B_c32])
            dma('sp', hvt, hv_d[l], [], [B_hv])
            rmask = c32[:, 0:2048]
            dmask4 = c32[:, 2048:2052]
            gsel = c32[:, 2052:2052 + 1024].rearrange("p (g s) -> p g s", s=128)
            hexp = c32[:, 3076:3076 + 2048].rearrange("p (b c) -> p b c", c=128)
            xr = [NT([128, 2052], BF16, 'xr%d' % i) for i in range(2)]
            dgs = [[NT([128, 128], BF16, 'dg%d_%d' % (i, k)) for k in range(4)] for i in range(2)]
            for i in range(2):
                memset('pool', xr[i][0][:, 0:4], 0.0, [xr[i][1]])
            base_off = aoff[0]

            def conv_block(src, wname, nb, blk, evac, keep_raw=None):
                s = nxt('xr', 2)
                xrt, xb = xr[s]
                dma('sp', xrt[:, 3:2051], src, [B_proj], [xb])
                dg = dgs[s]
                for k in range(4):
                    col = CVM[wname] + k * nb + blk
                    ts('pool', dg[k][0], identf[:], cvt[:, col:col + 1], ALU.mult, [B_c], [dg[k][1]])
                for ti in range(4):
                    pb = 5 + nxt('psm', 3)
                    for k in range(4):
                        mm(PS[pb][:], dg[k][0], xrt[:, ti * 512 + k:ti * 512 + k + 512], k == 0, k == 3,
                           [dg[k][1], xb], [PSB[pb]])
                    evac(ti, pb)
                return xrt, xb

            def lru():
                areset(base_off)
                c1, B_c1 = NT([128, 8], F32, 'c1')
                lam = cvt[:, CVM['lam']:CVM['lam'] + 8]
                act(c1, lam, AF.Exp, [B_c], [B_c1], scale=-1.0)
                act(c1, c1, AF.Ln, [B_c1], [B_c1], bias=1.0)
                ts('dve', c1, c1, -8.0, ALU.mult, [B_c1], [B_c1])
                wab = [NT([128, 128], BF16, 'wab%d' % i) for i in range(2)]
                wxb = [NT([128, 128], BF16, 'wxb%d' % i) for i in range(2)]
                xc32, B_xc = NT([128, 2048], F32, 'xc32')
                xcb, B_xcb = NT([128, 2048], BF16, 'xcb')
                r_, B_r = NT([128, 2048], F32, 'r')
                i_, B_i = NT([128, 2048], F32, 'i')
                a_, B_a = NT([128, 2048], F32, 'a')
                hs, B_hs = NT([128, 2048], F32, 'hs')
                lg, B_lg = NT([128, 2048], F32, 'lg')
                tmp, B_tmp = NT([128, 2048], F32, 'tmp')
                yo, B_yo = NT([128, 2048], BF16, 'yo')
                for b in range(8):
                    w = b % 2
                    dma('pool', wab[w][0], lbd_d[l, 0, b], [], [wab[w][1]])
                    dma('pool', wxb[w][0], lbd_d[l, 1, b], [], [wxb[w][1]])
                    bcol = CVM['lcb'] + b

                    def ev(ti, pb, bcol=bcol):
                        act(xc32[:, ti * 512:(ti + 1) * 512], PS[pb][:], AF.Identity, [PSB[pb]], [B_xc], bias=cvt[:, bcol:bcol + 1])
                    conv_block(lxT[b * 128:(b + 1) * 128, :], 'lcw', 8, b, ev)
                    if LSTOP == 1:
                        dma('sp', yT[2048 + b * 128:2048 + (b + 1) * 128, :], xr[0][0][:, 0:2048], [B_xc], [B_y])
                        continue
                    cp('dve', xcb, xc32, [B_xc], [B_xcb])
                    for ti in range(4):
                        sl = slice(ti * 512, (ti + 1) * 512)
                        pb = 5 + nxt('psm', 3)
                        mm(PS[pb][:], wab[w][0], xcb[:, sl], True, True, [wab[w][1], B_xcb], [PSB[pb]])
                        act(r_[:, sl], PS[pb][:], AF.Sigmoid, [PSB[pb]], [B_r], bias=cvt[:, CVM['lba'] + b:CVM['lba'] + b + 1])
                        pb = 5 + nxt('psm', 3)
                        mm(PS[pb][:], wxb[w][0], xcb[:, sl], True, True, [wxb[w][1], B_xcb], [PSB[pb]])
                        act(i_[:, sl], PS[pb][:], AF.Sigmoid, [PSB[pb]], [B_i], bias=cvt[:, CVM['lbx'] + b:CVM['lbx'] + b + 1])
                    if LSTOP == 2:
                        dma('sp', yT[2048 + b * 128:2048 + (b + 1) * 128, :], xcb, [B_r, B_i], [B_y])
                        continue
                    ts('dve', r_, r_, c1[:, b:b + 1], ALU.mult, [B_r, B_c1], [B_r])
                    act(a_, r_, AF.Exp, [B_r], [B_a])
                    act(r_, r_, AF.Exp, [B_r], [B_r], scale=2.0)
                    act(r_, r_, AF.Sqrt, [B_r], [B_r], scale=-1.0, bias=1.0)
                    tt('dve', i_, i_, xc32, ALU.mult, [B_i, B_xc], [B_i])
                    tt('dve', i_, i_, r_, ALU.mult, [B_i, B_r], [B_i])
                    if LSTOP == 3:
                        dma('sp', yT[2048 + b * 128:2048 + (b + 1) * 128, :], xcb, [B_a, B_i], [B_y])
                        continue
                    S.add('dve', lambda e: e.tensor_tensor_scan(out=hs, data0=a_, data1=i_, initial=0.0, op0=ALU.mult, op1=ALU.add),
                          [B_a, B_i], [B_hs])
                    dma('sp', lg, lgT[b * 128:(b + 1) * 128, :], [B_proj], [B_lg])
                    act(tmp, lg, AF.Square, [B_lg], [B_tmp])
                    ts('dve', tmp, tmp, 0.044715, ALU.mult, [B_tmp], [B_tmp], s2=1.0, op1=ALU.add)
                    tt('dve', tmp, tmp, lg, ALU.mult, [B_tmp, B_lg], [B_tmp])
                    act(tmp, tmp, AF.Sigmoid, [B_tmp], [B_tmp], scale=1.5957691216057308)
                    tt('dve', tmp, tmp, lg, ALU.mult, [B_tmp, B_lg], [B_tmp])
                    tt('dve', yo, hs, tmp, ALU.mult, [B_hs, B_tmp], [B_yo])
                    dma('sp', yT[2048 + b * 128:2048 + (b + 1) * 128, :], yo, [B_yo], [B_y])

            def ssd():
                areset(base_off)
                dtrF, B_dt = NT([128, 2048], F32, 'dtr')
                csTF, B_cs = NT([128, 2048], F32, 'csT')
                tmp32, B_t32 = NT([32, 2048], F32, 'tmp32')
                memset('dve', dtrF, 0.0, [B_dt])
                memset('dve', csTF, 0.0, [B_cs])
                dtr = dtrF[0:32, :]
                csT = csTF[0:32, :]
                rmask32 = rmask[0:32, :]
                aneg, B_an = NT([32, 1], F32, 'aneg')
                dt_tok, B_dtt = NT([128, 16, 32], F32, 'dt_tok')
                cs_tok, B_cst = NT([128, 16, 32], F32, 'cs_tok')
                ncs_tok, B_ncs = NT([128, 16, 32], F32, 'ncs_tok')
                w2_tok, B_w2 = NT([128, 16, 32], F32, 'w2_tok')
                te_tok, B_te = NT([128, 16, 32], F32, 'te_tok')
                cdB, B_cd = NT([128, 16, 32], F32, 'cdB')
                R16F, B_R16 = NT([128, 16, 32], F32, 'R16')
                memset('dve', R16F, 0.0, [B_R16])
                R16 = R16F[0:32]
                dma('sp', dtr, dtT, [B_proj], [B_dt])
                if SSTOP == 0.1:
                    return
                act(tmp32, dtr, AF.Exp, [B_dt, B_hv], [B_t32], bias=hvt[:, 0:1])
                act(dtr, tmp32, AF.Ln, [B_t32], [B_dt], bias=1.0)
                act(aneg, hvt[:, 1:2], AF.Exp, [B_hv], [B_an])
                ts('dve', aneg, aneg, -1.0, ALU.mult, [B_an], [B_an])
                ts('dve', tmp32, dtr, aneg[:, 0:1], ALU.mult, [B_dt, B_an], [B_t32])
                if SSTOP == 0.2:
                    return
                S.add('dve', lambda e: e.tensor_tensor_scan(out=csT, data0=rmask32, data1=tmp32, initial=0.0, op0=ALU.mult, op1=ALU.add),
                      [B_c32, B_t32], [B_cs])
                if SSTOP == 0.3:
                    return
                for c in range(16):
                    mm(PS[5][:, c * 32:(c + 1) * 32], dtrF[:, c * 128:(c + 1) * 128], identf[:, 0:32], True, True, [B_dt], [PSB[5]])
                    mm(PS[6][:, c * 32:(c + 1) * 32], csTF[:, c * 128:(c + 1) * 128], identf[:, 0:32], True, True, [B_cs], [PSB[6]])
                cp('act', dt_tok.rearrange("p c h -> p (c h)"), PS[5][:], [PSB[5]], [B_dtt])
                cp('dve', cs_tok.rearrange("p c h -> p (c h)"), PS[6][:], [PSB[6]], [B_cst])
                if SSTOP == 0.4:
                    return
                csend = csT.rearrange("p (c l) -> p c l", l=128)[:, :, 127]
                tt('dve', R16, csend.unsqueeze(2).to_broadcast([32, 16, 32]),
                   identf[0:32, 0:32].unsqueeze(1).to_broadcast([32, 16, 32]), ALU.mult, [B_cs, B_c], [B_R16])
                mm(PS[7][:], onesf[:], R16F.rearrange("p c h -> p (c h)"), True, True, [B_R16], [PSB[7]])
                act(cdB.rearrange("p c h -> p (c h)"), PS[7][:], AF.Exp, [PSB[7]], [B_cd])
                if SSTOP == 0.5:
                    return
                tt('dve', te_tok.rearrange("p c h -> p (c h)"), PS[7][:], cs_tok.rearrange("p c h -> p (c h)"), ALU.subtract,
                   [PSB[7], B_cst, B_cd], [B_te])
                if SSTOP == 0.6:
                    return
                act(te_tok, te_tok, AF.Exp, [B_te], [B_te])
                if SSTOP == 0.7:
                    return
                tt('dve', w2_tok, dt_tok, te_tok, ALU.mult, [B_dtt, B_te], [B_w2])
                ts('dve', ncs_tok, cs_tok, -1.0, ALU.mult, [B_cst], [B_ncs])
                if SSTOP == 1:
                    return
                xs32 = [(EV[i][:], EVB[i]) for i in range(2)]
                BT, B_BT = WS[0][:, 0:2048], Buf('BT')
                CT, B_CT = WS[0][:, 2048:4096], Buf('CT')
                Btok, B_Btok = NT([128, 16, 128], BF16, 'Btok')
                xd, B_xd = WS[1][:, 0:4096].rearrange('p (c x) -> p c x', x=256), Buf('xd')
                xdte, B_xdte = WS[2][:, 0:4096].rearrange('p (c x) -> p c x', x=256), Buf('xdte')
                Rc = [NT([128, 4, 128], F32, 'Rc%d' % i) for i in range(2)]
                for i in range(2):
                    memset('dve', Rc[i][0], 0.0, [Rc[i][1]])
                dec = [(SM[i][:].rearrange('p (j t) -> p j t', t=128), SMB[i]) for i in range(2)]
                MT = [NT([128, 4, 128], BF16, 'MT%d' % i) for i in range(2)]
                Hf, B_Hf = NT([128, 4, 64], F32, 'Hf')
                Hb, B_Hb = NT([128, 256], BF16, 'Hb')
                Ht, B_Ht = NT([128, 4, 64], F32, 'Ht')
                ecs, B_ecs = SM[2][:].rearrange('p (b t) -> p b t', t=256), SMB[2]
                t1, B_t1 = SM[3][:].rearrange('p (b t) -> p b t', t=256), SMB[3]
                zt, B_zt = SM[4][:].rearrange('p (b t) -> p b t', t=256), SMB[4]
                sq, B_sq = SM[5][:].rearrange('p (b t) -> p b t', t=256), SMB[5]
                rstd, B_rstd = RS[0][:, 0:256], RSB[0]
                yo2, B_yo2 = NT([128, 2, 256], BF16, 'yo2')
                B_sc = [PSB[0], PSB[7]]
                B_st = PSB[6]
                PSbf7 = PS[7][:].bitcast(BF16)
                for g in range(8):
                    for bl in range(2):
                        blk = 2 * g + bl
                        xst, xsb = xs32[bl]

                        def ev(ti, pb, xst=xst, xsb=xsb, blk=blk):
                            act(xst[:, ti * 512:(ti + 1) * 512], PS[pb][:], AF.Silu, [PSB[pb]], [xsb],
                                bias=cvt[:, CVM['scb'] + blk:CVM['scb'] + blk + 1])
                        conv_block(xbcT[blk * 128:(blk + 1) * 128, :], 'scw', 32, blk, ev)
                        for c4 in range(4):
                            pb = 5 + nxt('psm', 3)
                            for j in range(4):
                                c = c4 * 4 + j
                                tr(PS[pb][:, j * 128:(j + 1) * 128], xst[:, c * 128:(c + 1) * 128], identf[:], [xsb], [PSB[pb]])
                            pin = PS[pb][:].rearrange("p (c h x) -> p c h x", h=2, x=64)
                            for (dst, B_dst, wt, B_wt) in ((xd, B_xd, dt_tok, B_dtt), (xdte, B_xdte, w2_tok, B_w2)):
                                o = dst[:, c4 * 4:c4 * 4 + 4, bl * 128:(bl + 1) * 128].rearrange("p c (h x) -> p c h x", x=64)
                                wv_ = wt[:, c4 * 4:c4 * 4 + 4, 2 * blk:2 * blk + 2].unsqueeze(3).to_broadcast([128, 4, 2, 64])
                                tt('dve', o, pin, wv_, ALU.mult, [PSB[pb], B_wt], [B_dst])
                    for (dstT, B_dstT, blk) in ((BT, B_BT, 16 + g), (CT, B_CT, 24 + g)):
                        def ev(ti, pb, dstT=dstT, B_dstT=B_dstT, blk=blk):
                            act(dstT[:, ti * 512:(ti + 1) * 512], PS[pb][:], AF.Silu, [PSB[pb]], [B_dstT],
                                bias=cvt[:, CVM['scb'] + blk:CVM['scb'] + blk + 1])
                        conv_block(xbcT[blk * 128:(blk + 1) * 128, :], 'scw', 32, blk, ev)
                    for c8 in range(2):
                        for j in range(8):
                            c = c8 * 8 + j
                            tr(PSbf7[:, j * 128:(j + 1) * 128], BT[:, c * 128:(c + 1) * 128], identb[:], [B_BT], [PSB[7]])
                        cp('act', Btok[:, c8 * 8:(c8 + 1) * 8, :].rearrange("p c n -> p (c n)"), PSbf7[:, 0:1024], [PSB[7]], [B_Btok])
                    if SSTOP == 2:
                        return
                    memset('dve', Hf, 0.0, [B_Hf])
                    memset('dve', Hb, 0.0, [B_Hb])

                    def partA(c):
                        p = c % 2
                        sc = PS[0][:, 0:128] if p == 0 else PS[7][:, 0:128]
                        mm(sc, BT[:, c * 128:(c + 1) * 128], CT[:, c * 128:(c + 1) * 128], True, True, [B_BT, B_CT], [B_sc[p]])
                        rc, B_rc = Rc[p]
                        tt('dve', rc[0:32], csT[:, c * 128:(c + 1) * 128].unsqueeze(1).to_broadcast([32, 4, 128]),
                           dmask4[0:32].unsqueeze(2).to_broadcast([32, 4, 128]), ALU.mult, [B_cs, B_c32], [B_rc])
                        pd = 1 + p
                        mm(PS[pd][:], identb[:], mneg[:], True, False, [B_c], [PSB[pd]])
                        mm(PS[pd][:], gsel[:, g, :], rc.rearrange("p j t -> p (j t)"), False, True, [B_c32, B_rc], [PSB[pd]])

                    def chain(c):
                        p = c % 2
                        sc = PS[0][:, 0:128] if p == 0 else PS[7][:, 0:128]
                        d_, B_d = dec[p]
                        m_, B_m = MT[p]
                        pd = 1 + p
                        for j in range(4):
                            h = 4 * g + j
                            act(d_[:, j, :], PS[pd][:, j * 128:(j + 1) * 128], AF.Exp, [PSB[pd], B_ncs], [B_d],
                                bias=ncs_tok[:, c, h:h + 1])
                        tt('dve', m_, d_, sc.unsqueeze(1).to_broadcast([128, 4, 128]), ALU.mult, [B_d, B_sc[p]], [B_m])

                    def partB(c):
                        p = c % 2
                        cc = c % 2
                        m_, B_m = MT[p]
                        for j in range(4):
                            po = (j % 2) * 64
                            col = (j // 2) * 256 + cc * 128
                            mm(PS[3][po:po + 64, col:col + 128], xd[:, c, j * 64:(j + 1) * 64], m_[:, j, :], True, True,
                               [B_xd, B_m], [PSB[3]])
                        for j in range(4):
                            po = (j % 2) * 64
                            col = (j // 2) * 256 + cc * 128
                            mm(PS[4][po:po + 64, col:col + 128], Hb[:, j * 64:(j + 1) * 64], CT[:, c * 128:(c + 1) * 128], True, True,
                               [B_Hb, B_CT], [PSB[4]])
                        mm(PS[6][:, 0:256], Btok[:, c, :], xdte[:, c, :], True, True, [B_Btok, B_xdte], [B_st])

                    def state_update(c):
                        tt('dve', Ht, Hf, cdB[:, c, 4 * g:4 * g + 4].unsqueeze(2).to_broadcast([128, 4, 64]), ALU.mult,
                           [B_Hf, B_cd], [B_Ht])
                        tt('dve', Hf, Ht, PS[6][:, 0:256].rearrange("p (j x) -> p j x", x=64), ALU.add, [B_Ht, B_st], [B_Hf])
                        cp('act', Hb, Hf.rearrange("p j x -> p (j x)"), [B_Hf], [B_Hb])

                    t1s = [(t1, B_t1), (RS[1][:].rearrange('p (b t) -> p b t', t=256), RSB[1])]

                    def combine1(c):
                        t0 = (c - 1) * 128
                        t1, B_t1 = t1s[(c // 2) % 2]
                        for bl in range(2):
                            mm(PS[5][:, bl * 256:(bl + 1) * 256], hexp[:, 2 * g + bl, :], csTF[:, t0:t0 + 256], True, True,
                               [B_c32, B_cs], [PSB[5]])
                        act(ecs.rearrange("p b t -> p (b t)"), PS[5][:], AF.Exp, [PSB[5]], [B_ecs])
                        tt('dve', t1.rearrange("p b t -> p (b t)"), PS[4][:], ecs.rearrange("p b t -> p (b t)"), ALU.mult,
                           [PSB[4], B_ecs], [B_t1])
                        tt('dve', t1.rearrange("p b t -> p (b t)"), t1.rearrange("p b t -> p (b t)"), PS[3][:], ALU.add,
                           [B_t1, PSB[3]], [B_t1])

                    def combine2(c):
                        t0 = (c - 1) * 128
                        t1, B_t1 = t1s[(c // 2) % 2]
                        for bl in range(2):
                            blk = 2 * g + bl
                            stt(t1[:, bl, :], xs32[bl][0][:, t0:t0 + 256], cvt[:, CVM['sd'] + blk:CVM['sd'] + blk + 1], t1[:, bl, :],
                                ALU.mult, ALU.add, [xs32[bl][1], B_t1], [B_t1])
                        dma('sp', zt, zT[2 * g * 128:(2 * g + 2) * 128, t0:t0 + 256].rearrange("(b p) t -> p b t", p=128), [B_proj], [B_zt])
                        act(zt, zt, AF.Silu, [B_zt], [B_zt])
                        tt('dve', t1, t1, zt, ALU.mult, [B_t1, B_zt], [B_t1])
                        act(sq, t1, AF.Square, [B_t1], [B_sq])
                        mm(PS[5][:, 0:256], onesf[:], sq[:, 0, :], True, False, [B_sq], [PSB[5]])
                        mm(PS[5][:, 0:256], onesf[:], sq[:, 1, :], False, True, [B_sq], [PSB[5]])
                        act(rstd, PS[5][:, 0:256], AF.Sqrt, [PSB[5]], [B_rstd], scale=1.0 / 256, bias=EPS)
                        recip(rstd, rstd, [B_rstd], [B_rstd])
                        for bl in range(2):
                            blk = 2 * g + bl
                            stt(yo2[:, bl, :], t1[:, bl, :], cvt[:, CVM['snw'] + blk:CVM['snw'] + blk + 1], rstd, ALU.mult, ALU.mult,
                                [B_t1, B_rstd], [B_yo2])
                        dma('sp', yT[2 * g * 128:(2 * g + 2) * 128, t0:t0 + 256].rearrange("(b p) t -> p b t", p=128), yo2, [B_yo2], [B_y])

                    partA(0)
                    chain(0)
                    for c in range(16):
                        if c + 1 < 16:
                            partA(c + 1)
                        partB(c)
                        if c + 1 < 16:
                            chain(c + 1)
                        state_update(c)
                        if c % 2 == 1:
                            combine1(c)
                        if c % 2 == 0 and c >= 2:
                            combine2(c - 1)
                    combine2(15)
                    if SSTOP == 6:
                        return

            def mlstm():
                areset(base_off)
                wifb, B_wif = NT([128, 24, 8], BF16, 'wifb')
                dma('pool', wifb, wif_d[l], [], [B_wif])
                bd = [[NT([128, 128], BF16, 'bd%d_%d' % (i, s_)) for s_ in range(2)] for i in range(3)]
                G = [NT([128, 2048], F32, 'G%d' % i) for i in range(4)]
                for i in range(4):
                    memset('dve', G[i][0], 0.0, [G[i][1]])
                q_sb, B_q = WS[0][:, 0:2048], Buf('q_sb')
                qs_sb, B_qs = WS[0][:, 2048:4096], Buf('qs_sb')
                k_sb, B_k = WS[1][:, 0:2048], Buf('k_sb')
                v_sb, B_v = WS[1][:, 2048:4096], Buf('v_sb')
                mc, B_mc = WS[2][:, 0:2048], Buf('mc')
                for b in range(8):
                    s_ = b % 2
                    for i in range(3):
                        dma('pool', bd[i][s_][0], bdq_d[l, i, b], [], [bd[i][s_][1]])

                    def ev(ti, pb, b=b):
                        act(mc[:, ti * 512:(ti + 1) * 512], PS[pb][:], AF.Silu, [PSB[pb]], [B_mc],
                            bias=cvt[:, CVM['mcb'] + b:CVM['mcb'] + b + 1])
                    xrt, xb = conv_block(mxT[b * 128:(b + 1) * 128, :], 'mcw', 8, b, ev)
                    for ti in range(4):
                        sl = slice(ti * 512, (ti + 1) * 512)
                        pb = 5 + nxt('psm', 3)
                        mm(PS[pb][:], bd[0][s_][0], mc[:, sl], True, True, [bd[0][s_][1], B_mc], [PSB[pb]])
                        act(q_sb[:, sl], PS[pb][:], AF.Copy, [PSB[pb]], [B_q])
                        act(qs_sb[:, sl], PS[pb][:], AF.Copy, [PSB[pb]], [B_qs], scale=1.0 / 16)
                        pb = 5 + nxt('psm', 3)
                        mm(PS[pb][:], bd[1][s_][0], mc[:, sl], True, True, [bd[1][s_][1], B_mc], [PSB[pb]])
                        cp('dve', k_sb[:, sl], PS[pb][:], [PSB[pb]], [B_k])
                        pb = 5 + nxt('psm', 3)
                        mm(PS[pb][:], bd[2][s_][0], xrt[:, 3 + ti * 512:3 + (ti + 1) * 512], True, True, [bd[2][s_][1], xb], [PSB[pb]])
                        cp('dve', v_sb[:, sl], PS[pb][:], [PSB[pb]], [B_v])
                        for i, (src, B_src) in enumerate(((q_sb, B_q), (k_sb, B_k), (v_sb, B_v))):
                            mm(PS[ti][0:8, :], wifb[:, i * 8 + b, :], src[:, sl], (b == 0 and i == 0), (b == 7 and i == 2),
                               [B_wif, B_src], [PSB[ti]])
                    dma('sp', mq_d[b * 128:(b + 1) * 128, :], qs_sb, [B_qs], [B_mq])
                    dma('sp', mk_d[b * 128:(b + 1) * 128, :], k_sb, [B_k], [B_mq])
                    dma('sp', mv_d[b * 128:(b + 1) * 128, :], v_sb, [B_v], [B_mq])
                G0, B_G0 = G[0]; G1, B_G1 = G[1]; G2, B_G2 = G[2]; G3, B_G3 = G[3]
                for ti in range(4):
                    cp('act', G0[0:8, ti * 512:(ti + 1) * 512], PS[ti][0:8, :], [PSB[ti]], [B_G0])
                sm_, B_sm = NT([4, 160], F32, 'mlsmall')
                nbf = sm_[:, 0:1]; gmax = sm_[:, 16:32]; mloc = sm_[:, 32:48]; mm_ = sm_[:, 48:65]
                sold = sm_[:, 80:96]; sloc = sm_[:, 96:112]; tmp16 = sm_[:, 112:128]
                ts('dve', nbf, hvt[0:4, 3:4], -1.0, ALU.mult, [B_hv], [B_sm])
                for ti in range(4):
                    sl = slice(ti * 512, (ti + 1) * 512)
                    pb = 5 + nxt('psm', 3)
                    mm(PS[pb][0:4, :], identf[:, 4:8], G0[:, sl], True, True, [B_G0], [PSB[pb]])
                    act(G1[0:4, sl], PS[pb][0:4, :], AF.Exp, [PSB[pb], B_sm], [B_G1], scale=-1.0, bias=nbf)
                act(G1[0:4, :], G1[0:4, :], AF.Ln, [B_G1], [B_G1], bias=1.0)
                ts('dve', G1[0:4, :], G1[0:4, :], -1.0, ALU.mult, [B_G1], [B_G1])
                S.add('dve', lambda e: e.tensor_tensor_scan(out=G2[0:4, :], data0=rmask[0:4, :], data1=G1[0:4, :], initial=0.0,
                                                            op0=ALU.mult, op1=ALU.add), [B_c32, B_G1], [B_G2])
                ts('dve', G0[0:4, :], G0[0:4, :], hvt[0:4, 2:3], ALU.add, [B_G0, B_hv], [B_G0])
                tt('dve', G1[0:4, :], G0[0:4, :], G2[0:4, :], ALU.subtract, [B_G0, B_G2], [B_G1])
                g3 = G1[0:4, :].rearrange("p (c l) -> p c l", l=128)
                S.add('dve', lambda e: e.tensor_reduce(out=gmax, in_=g3, axis=AX.X, op=ALU.max), [B_G1], [B_sm])
                gtot = G2[0:4, :].rearrange("p (c l) -> p c l", l=128)[:, :, 127]
                tt('dve', mloc, gtot, gmax, ALU.add, [B_G2, B_sm], [B_sm])
                memset('dve', mm_[:, 0:1], 0.0, [B_sm])
                for c in range(16):
                    stt(mm_[:, c + 1:c + 2], gtot[:, c:c + 1], mm_[:, c:c + 1], mloc[:, c:c + 1], ALU.add, ALU.max,
                        [B_G2, B_sm], [B_sm])
                for c in range(16):
                    S.add('dve', (lambda c: (lambda e: e.tensor_tensor_scan(out=G0[0:4, c * 128:(c + 1) * 128], data0=onesf[0:4, :],
                                                                            data1=G1[0:4, c * 128:(c + 1) * 128], initial=mm_[:, c:c + 1],
                                                                            op0=ALU.mult, op1=ALU.max)))(c), [B_G1, B_sm], [B_G0])
                tt('dve', tmp16, gtot, mm_[:, 0:16], ALU.add, [B_G2, B_sm], [B_sm])
                tt('dve', tmp16, tmp16, mm_[:, 1:17], ALU.subtract, [B_sm], [B_sm])
                act(sold, tmp16, AF.Exp, [B_sm], [B_sm])
                tt('dve', tmp16, mloc, mm_[:, 1:17], ALU.subtract, [B_sm], [B_sm])
                act(sloc, tmp16, AF.Exp, [B_sm], [B_sm])
                tok4, B_tok4 = NT([128, 4, 16, 4], F32, 'tok4')
                ssB, B_ssB = NT([128, 2, 16, 4], F32, 'ssB')
                R2F, B_R2 = NT([128, 2, 16, 4], F32, 'R2')
                memset('dve', R2F, 0.0, [B_R2])
                R2 = R2F[0:4]
                eye4 = identf[0:4, 0:4]

                def tok_tr(q, X, B_X):
                    for c in range(16):
                        mm(PS[5][:, q * 64 + c * 4:q * 64 + c * 4 + 4], X[:, c * 128:(c + 1) * 128], identf[:, 0:4], True, True, [B_X], [PSB[5]])
                tok_tr(0, G1, B_G1)
                M3 = G0[0:4, :].rearrange("p (c l) -> p c l", l=128)
                G33 = G3[0:4, :].rearrange("p (c l) -> p c l", l=128)
                tt('dve', G33, g3, gmax.unsqueeze(2).to_broadcast([4, 16, 128]), ALU.subtract, [B_G1, B_sm], [B_G3])
                act(G3[0:4, :], G3[0:4, :], AF.Exp, [B_G3], [B_G3])
                tok_tr(1, G3, B_G3)
                tt('dve', G33, mm_[:, 0:16].unsqueeze(2).to_broadcast([4, 16, 128]), M3, ALU.subtract, [B_sm, B_G0], [B_G3])
                act(G3[0:4, :], G3[0:4, :], AF.Exp, [B_G3], [B_G3])
                tok_tr(2, G3, B_G3)
                tt('dve', G3[0:4, :], G2[0:4, :], G0[0:4, :], ALU.add, [B_G2, B_G0], [B_G3])
                act(G3[0:4, :], G3[0:4, :], AF.Exp, [B_G3], [B_G3], scale=-1.0)
                tok_tr(3, G3, B_G3)
                cp('act', tok4.rearrange("p q c h -> p (q c h)"), PS[5][:, 0:256], [PSB[5]], [B_tok4])
                ts('dve', G2[0:4, :], G0[0:4, :], -1.0, ALU.mult, [B_G0], [B_G2])
                for i, sv in enumerate((sold, sloc)):
                    tt('dve', R2[:, i], sv.unsqueeze(2).to_broadcast([4, 16, 4]), eye4.unsqueeze(1).to_broadcast([4, 16, 4]),
                       ALU.mult, [B_sm, B_c], [B_R2])
                mm(PS[6][:, 0:128], onesf[:], R2F.rearrange("p i c h -> p (i c h)"), True, True, [B_R2], [PSB[6]])
                cp('act', ssB.rearrange("p i c h -> p (i c h)"), PS[6][:, 0:128], [PSB[6]], [B_ssB])
                S.barrier()
                ktok, B_ktok = NT([128, 16, 256], BF16, 'ktok')
                vx, B_vx = NT([128, 16, 258], BF16, 'vx')
                vpe, B_vpe = NT([128, 16, 258], BF16, 'vpe')
                ybuf, B_yb = NT([128, 2, 2048], BF16, 'ybuf')
                CTf, B_CTf = NT([128, 2, 258], F32, 'CTf')
                CTb, B_CTb = NT([128, 2, 258], BF16, 'CTb')
                PT, B_PT = SM[0][:, 0:128], SMB[0]
                qkT, B_qkT = NT([128, 128], BF16, 'qkT')
                tb, B_tb = SM[1][:, 0:257], SMB[1]
                tot, B_tot = SM[2][:, 0:257], SMB[2]
                hh, B_hh = SM[3][:, 0:256], SMB[3]
                st6, B_st6 = NT([128, 8], F32, 'st6')
                mv, B_mv = NT([128, 2], F32, 'mv')
                qsT, B_qsT = WS[0][:, 0:4096].rearrange("p (b t) -> p b t", t=2048), Buf('qsT')
                kT, B_kT = WS[1][:, 0:4096].rearrange("p (b t) -> p b t", t=2048), Buf('kT')
                vT, B_vT = WS[2][:, 0:4096].rearrange("p (b t) -> p b t", t=2048), Buf('vT')
                sigo = [(EV[i][:], EVB[i]) for i in range(2)]
                PSbf7 = PS[7][:].bitcast(BF16)
                memset('dve', vx[:, :, 256:258], 1.0, [B_vx])
                for h in range(4):
                    for (dst, B_dst, src) in ((qsT, B_qsT, mq_d), (kT, B_kT, mk_d), (vT, B_vT, mv_d)):
                        dma('sp', dst, src[h * 256:(h + 1) * 256, :].rearrange("(b p) t -> p b t", p=128), [B_mq], [B_dst])
                    for bl in range(2):
                        dma('sp', sigo[bl][0], moT[(2 * h + bl) * 128:(2 * h + bl + 1) * 128, :], [B_proj], [sigo[bl][1]])
                        act(sigo[bl][0], sigo[bl][0], AF.Sigmoid, [sigo[bl][1]], [sigo[bl][1]])
                    for bl in range(2):
                        for c8 in range(2):
                            for j in range(8):
                                c = c8 * 8 + j
                                tr(PSbf7[:, j * 128:(j + 1) * 128], kT[:, bl, c * 128:(c + 1) * 128], identb[:], [B_kT], [PSB[7]])
                            cp('act', ktok[:, c8 * 8:(c8 + 1) * 8, bl * 128:(bl + 1) * 128],
                               PSbf7[:, 0:1024].rearrange("p (c n) -> p c n", n=128), [PSB[7]], [B_ktok])
                            for j in range(8):
                                c = c8 * 8 + j
                                tr(PSbf7[:, j * 128:(j + 1) * 128], vT[:, bl, c * 128:(c + 1) * 128], identb[:], [B_vT], [PSB[7]])
                            pv = PSbf7[:, 0:1024].rearrange("p (c n) -> p c n", n=128)
                            cp('act', vx[:, c8 * 8:(c8 + 1) * 8, bl * 128:(bl + 1) * 128], pv, [PSB[7]], [B_vx])
                            tt('dve', vpe[:, c8 * 8:(c8 + 1) * 8, bl * 128:(bl + 1) * 128], pv,
                               tok4[:, 1, c8 * 8:(c8 + 1) * 8, h:h + 1].to_broadcast([128, 8, 128]), ALU.mult, [PSB[7], B_tok4], [B_vpe])
                    for j in range(2):
                        cp('dve', vpe[:, :, 256 + j], tok4[:, 1, :, h], [B_tok4], [B_vpe])
                    ts('dve', G3[0:4, :], G2[0:4, :], identf[0:4, h:h + 1], ALU.mult, [B_G2, B_c], [B_G3])
                    memset('dve', CTf, 0.0, [B_CTf])
                    memset('dve', CTb, 0.0, [B_CTb])
                    def front_a(c):
                        cs_ = slice(c * 128, (c + 1) * 128)
                        mm(PS[0][:, 0:128], kT[:, 0, cs_], qsT[:, 0, cs_], True, False, [B_kT, B_qsT], [PSB[0]])
                        mm(PS[0][:, 0:128], kT[:, 1, cs_], qsT[:, 1, cs_], False, True, [B_kT, B_qsT], [PSB[0]])
                        mm(PS[1][:, 0:128], identb[:], mneg[:, 0:128], True, False, [B_c], [PSB[1]])
                        mm(PS[1][:, 0:128], onesf[:], G3[:, cs_], False, True, [B_G3], [PSB[1]])
                        act(PT, PS[1][:, 0:128], AF.Exp, [PSB[1], B_tok4], [B_PT], bias=tok4[:, 0, c, h:h + 1])

                    def front_b(c):
                        cs_ = slice(c * 128, (c + 1) * 128)
                        tt('dve', qkT, PT, PS[0][:, 0:128], ALU.mult, [B_PT, PSB[0]], [B_qkT])
                        mm(PS[2][:, 0:257], qkT, vx[:, c, 0:257], True, True, [B_qkT, B_vx], [PSB[2]])
                        mm(PS[3][:, 0:257], qsT[:, 0, cs_], CTb[:, 0, 0:257], True, False, [B_qsT, B_CTb], [PSB[3]])
                        mm(PS[3][:, 0:257], qsT[:, 1, cs_], CTb[:, 1, 0:257], False, True, [B_qsT, B_CTb], [PSB[3]])

                    def mid(c):
                        ts('dve', tb, PS[3][:, 0:257], tok4[:, 2, c, h:h + 1], ALU.mult, [PSB[3], B_tok4], [B_tb])
                        tt('dve', tot, tb, PS[2][:, 0:257], ALU.add, [B_tb, PSB[2]], [B_tot])

                    def state(c):
                        for eb in range(2):
                            mm(PS[5 + eb][:, 0:257], ktok[:, c, eb * 128:(eb + 1) * 128], vpe[:, c, 0:257], True, True,
                               [B_ktok, B_vpe], [PSB[5 + eb]])
                        for eb in range(2):
                            ts('dve', CTf[:, eb, 0:257], CTf[:, eb, 0:257], ssB[:, 0, c, h:h + 1], ALU.mult, [B_CTf, B_ssB], [B_CTf])
                            stt(CTf[:, eb, 0:257], PS[5 + eb][:, 0:257], ssB[:, 1, c, h:h + 1], CTf[:, eb, 0:257], ALU.mult, ALU.add,
                                [PSB[5 + eb], B_ssB, B_CTf], [B_CTf])
                        cp('act', CTb[:, :, 0:257], CTf[:, :, 0:257], [B_CTf], [B_CTb])

                    def tail_1(c):
                        ts('dve', st6[:, 7:8], tot[:, 256:257], -1.0, ALU.mult, [B_tot], [B_st6])
                        tt('dve', st6[:, 6:7], tot[:, 256:257], st6[:, 7:8], ALU.max, [B_tot, B_st6], [B_st6])
                        tt('dve', st6[:, 6:7], st6[:, 6:7], tok4[:, 3, c, h:h + 1], ALU.max, [B_st6, B_tok4], [B_st6])
                        recip(st6[:, 6:7], st6[:, 6:7], [B_st6], [B_st6])
                        ts('dve', hh, tot[:, 0:256], st6[:, 6:7], ALU.mult, [B_tot, B_st6], [B_hh])
                        S.add('dve', lambda e: e.bn_stats(out=st6[:, 0:6], in_=hh), [B_hh], [B_st6])
                        S.add('dve', lambda e: e.bn_aggr(out=mv, in_=st6[:, 0:6]), [B_st6], [B_mv])
                        act(mv[:, 1:2], mv[:, 1:2], AF.Sqrt, [B_mv], [B_mv], bias=EPS)

                    def tail_2(c):
                        cs_ = slice(c * 128, (c + 1) * 128)
                        recip(mv[:, 1:2], mv[:, 1:2], [B_mv], [B_mv])
                        ts('dve', hh, hh, mv[:, 0:1], ALU.subtract, [B_hh, B_mv], [B_hh], s2=mv[:, 1:2], op1=ALU.mult)
                        for bl in range(2):
                            tr(PS[4][:, bl * 128:(bl + 1) * 128], hh[:, bl * 128:(bl + 1) * 128], identf[:], [B_hh], [PSB[4]])
                            col = CVM['mnw'] + 2 * h + bl
                            stt(ybuf[:, bl, cs_], PS[4][:, bl * 128:(bl + 1) * 128], cvt[:, col:col + 1], sigo[bl][0][:, cs_],
                                ALU.mult, ALU.mult, [PSB[4], sigo[bl][1]], [B_yb])

                    front_a(0)
                    front_b(0)
                    for c in range(16):
                        mid(c)
                        state(c)
                        if c + 1 < 16:
                            front_a(c + 1)
                        tail_1(c)
                        if c + 1 < 16:
                            front_b(c + 1)
                        tail_2(c)
                    dma('sp', yT[3072 + h * 256:3072 + (h + 1) * 256, :].rearrange("(b p) t -> p b t", p=128), ybuf, [B_yb], [B_y])

            if 'lru' in mixers:
                lru()
                S.barrier()
            if 'ssd' in mixers:
                ssd()
                S.barrier()
            if 'ml' in mixers:
                mlstm()
                S.barrier()

        if mixtest:
            mixer_stage(0)
            nl = 0
        else:
            stage0()
            S.barrier()
        for l in range(nl):
            uT = arv(0, [128, 16, 2048], BF16)
            B_u = Buf('uT')
            norm_stage(cvs[l], CVM['n1'], uT, B_u, 0, 2048)
            proj_stage(l, uT, B_u)
            S.barrier()
            mixer_stage(l)
            S.barrier()
            wout_stage(l)
            if do_ffn:
                ffn_stage(l)
        if not mixtest:
            final_stage()
        S.emit()
    return nc, S


def host_prep(inputs):
    f = lambda k: np.asarray(inputs[k], dtype=np.float32)
    cv = np.zeros((NL, 128, NCV), np.float32)

    def put(l, name, vec, nblk):
        cv[l, :, CVM[name]:CVM[name] + nblk] = vec.reshape(nblk, 128).T
    for l in range(NL):
        put(l, 'n1', f('norm1_w')[l], 16)
        put(l, 'n2', f('norm2_w')[l], 16)
        scw = f('ssd_conv_w')[l]
        for k in range(4):
            cv[l, :, CVM['scw'] + k * 32:CVM['scw'] + (k + 1) * 32] = scw[k].reshape(32, 128).T
        put(l, 'scb', f('ssd_conv_b')[l], 32)
        put(l, 'sd', np.repeat(f('ssd_d')[l], 64), 16)
        put(l, 'snw', f('ssd_norm_w')[l], 16)
        lcw = f('lru_conv_w')[l]
        for k in range(4):
            cv[l, :, CVM['lcw'] + k * 8:CVM['lcw'] + (k + 1) * 8] = lcw[k].reshape(8, 128).T
        put(l, 'lcb', f('lru_conv_b')[l], 8)
        put(l, 'lba', f('lru_b_a')[l], 8)
        put(l, 'lbx', f('lru_b_x')[l], 8)
        put(l, 'lam', f('lru_lambda')[l], 8)
        mcw = f('ml_conv_w')[l]
        for k in range(4):
            cv[l, :, CVM['mcw'] + k * 8:CVM['mcw'] + (k + 1) * 8] = mcw[k].reshape(8, 128).T
        put(l, 'mcb', f('ml_conv_b')[l], 8)
        put(l, 'mnw', f('ml_norm_w')[l], 8)
        put(l, 'nf', f('norm_f_w'), 16)
    hv = np.zeros((NL, 32, 4), np.float32)
    hv[:, :, 0] = f('ssd_dt_bias')
    hv[:, :, 1] = f('ssd_a_log')
    hv[:, 0:4, 2] = f('ml_b_if')[:, 0:4]
    hv[:, 0:4, 3] = f('ml_b_if')[:, 4:8]
    bdq = np.zeros((NL, 3, 8, 128, 128), np.float32)
    for i, nm in enumerate(['ml_w_q', 'ml_w_k', 'ml_w_v']):
        w = f(nm)
        for n in range(256):
            b, o = divmod(n, 32)
            bdq[:, i, b, o * 4:o * 4 + 4, o * 4:o * 4 + 4] = w[:, n]
    lbd = np.zeros((NL, 2, 8, 128, 128), np.float32)
    for i, nm in enumerate(['lru_w_a', 'lru_w_x']):
        w = f(nm)
        for n in range(16):
            b, o = divmod(n, 2)
            lbd[:, i, b, o * 64:o * 64 + 64, o * 64:o * 64 + 64] = w[:, n]
    wif = np.ascontiguousarray(f('ml_w_if').reshape(NL, 24, 128, 8).transpose(0, 2, 1, 3))
    cst = np.zeros((128, 640), np.float32)
    cst[:, 0:128] = np.eye(128, dtype=np.float32)
    s_ = np.arange(128)[:, None]
    t_ = np.arange(128)[None, :]
    mk = np.where(s_ <= t_, 0.0, NEG).astype(np.float32)
    cst[:, 128:640] = np.tile(mk, (1, 4))
    c32 = np.zeros((128, 2048 + 4 + 1024 + 2048), np.float32)
    rm = np.ones(2048, np.float32)
    rm[::128] = 0.0
    c32[0:32, 0:2048] = rm[None, :]
    hh = np.arange(32)
    for j in range(4):
        c32[0:32, 2048 + j] = (hh % 4 == j)
    for g in range(8):
        c32[0:32, 2052 + g * 128:2052 + (g + 1) * 128] = (hh // 4 == g)[:, None]
    for b in range(16):
        ch = np.arange(128)
        c32[0:32, 3076 + b * 128:3076 + (b + 1) * 128] = (hh[:, None] == (2 * b + ch[None, :] // 64))
    shared = {"w_in": f('w_in'), "w_out": f('w_out'), "w_gate_up": f('w_gate_up'), "w_down": f('w_down'),
              "cv": cv, "hv": hv, "bdq": bdq, "lbd": lbd, "wif": wif, "cst": cst, "c32": c32,
              "nfb": f('norm_f_w').reshape(1, D)}
    return shared


_CACHE = {}


def kernel(**inputs):
    shared = host_prep(inputs)
    if 'nc' not in _CACHE:
        _CACHE['nc'] = build()[0]
    nc = _CACHE['nc']
    xs = np.asarray(inputs['x'], dtype=np.float32)
    in_maps = []
    for b in range(8):
        m = dict(shared)
        m["x"] = np.ascontiguousarray(xs[b])
        in_maps.append(m)
    res = run_bass_kernel_spmd(nc, in_maps, core_ids=list(range(8)))
    return np.stack([np.asarray(r["out"]) for r in res.results], axis=0).astype(np.float32)
```

```python
import contextlib
import numpy as np
import concourse.bass as bass
import concourse.mybir as mybir
from concourse.bass_utils import run_bass_kernel_spmd

F32 = mybir.dt.float32
BF16 = mybir.dt.bfloat16
AF = mybir.ActivationFunctionType
ALU = mybir.AluOpType
AX = mybir.AxisListType

T = 2048
D = 2048
DIN = 10272
DFF = 5632
NL = 2
EPS = 1e-6
NEG = -30000.0
import os
LSTOP = int(os.environ.get('LSTOP', '0'))
SSTOP = float(os.environ.get('SSTOP', '0'))
ENGS = ['pe', 'act', 'dve', 'pool', 'sp']


class Buf:
    __slots__ = ('name', 'lw', 'rd', 'const', 'excl')

    def __init__(self, name, const=False, excl=False):
        self.name = name
        self.lw = None
        self.rd = []
        self.const = const
        self.excl = excl


class Op:
    __slots__ = ('eng', 'fn', 'deps', 'sig', 'dma', 'slot', 'sem', 'val', 'n')


class Sched:
    def __init__(self, nc, ndma=8):
        self.nc = nc
        self.ops = {e: [] for e in ENGS}
        self.ndma = ndma
        self.dma_cnt = {e: 0 for e in ENGS}
        self.dma_last = {}
        self.nops = 0

    def add(self, eng, fn, rd=(), wr=(), dma=False):
        op = Op()
        op.eng = eng
        op.fn = fn
        op.dma = dma
        op.sig = False
        op.n = self.nops
        self.nops += 1
        ex = [b for b in rd if b.excl]
        if ex:
            wr = list(wr) + [b for b in ex if all(b is not w for w in wr)]
        deps = {}
        for b in rd:
            if b.lw is not None:
                deps[id(b.lw)] = b.lw
        for b in wr:
            if b.lw is not None:
                deps[id(b.lw)] = b.lw
            for r in b.rd:
                deps[id(r)] = r
        if dma:
            slot = self.dma_cnt[eng] % self.ndma
            self.dma_cnt[eng] += 1
            op.slot = slot
            prev = self.dma_last.get((eng, slot))
            if prev is not None:
                deps[id(prev)] = prev
            self.dma_last[(eng, slot)] = op
        dl = []
        for d in deps.values():
            if eng == 'pe' and d.eng == 'pe' and not d.dma and not dma:
                continue
            dl.append(d)
        op.deps = dl
        wrs = set(id(b) for b in wr)
        for b in rd:
            if not b.const and id(b) not in wrs:
                b.rd.append(op)
        for b in wr:
            b.lw = op
            b.rd = []
        self.ops[eng].append(op)
        return op

    def barrier(self):
        lasts = [self.ops[e][-1] for e in ENGS if self.ops[e]] + list(self.dma_last.values())
        for e in ENGS:
            op = Op()
            op.eng = e; op.fn = None; op.dma = False; op.sig = False; op.n = self.nops
            op.deps = [d for d in lasts if d.fn is not None]
            self.ops[e].append(op)

    def emit(self):
        nc = self.nc
        fin = Op()
        fin.eng = 'sp'; fin.fn = None; fin.dma = False; fin.sig = False
        fin.deps = list(self.dma_last.values()); fin.n = self.nops
        self.ops['sp'].append(fin)
        for e in ENGS:
            for op in self.ops[e]:
                for d in op.deps:
                    d.sig = True
        with contextlib.ExitStack() as st:
            csem = {e: st.enter_context(nc.semaphore('c_' + e)) for e in ENGS}
            dsem = {}
            for e in ENGS:
                for j in range(min(self.ndma, self.dma_cnt[e])):
                    dsem[(e, j)] = st.enter_context(nc.semaphore('d_%s%d' % (e, j)))
            for e in ENGS:
                cnt = 0
                dcnt = {}
                for op in self.ops[e]:
                    if op.dma:
                        dcnt[op.slot] = dcnt.get(op.slot, 0) + 1
                        op.sem = dsem[(e, op.slot)]
                        op.val = 16 * dcnt[op.slot]
                    elif op.sig:
                        cnt += 1
                        op.sem = csem[e]
                        op.val = cnt
            block = st.enter_context(nc.Block())
            stats = {}

            def run(e, eng):
                waited = {}
                nw = 0
                for op in self.ops[e]:
                    need = {}
                    for d in op.deps:
                        k = id(d.sem)
                        if waited.get(k, 0) >= d.val:
                            continue
                        if k not in need or need[k][1] < d.val:
                            need[k] = (d.sem, d.val)
                    for k, (sem, val) in need.items():
                        eng.wait_ge(sem, val)
                        waited[k] = val
                        nw += 1
                    if op.fn is None:
                        continue
                    ins = op.fn(eng)
                    if op.dma:
                        ins.then_inc(op.sem, 16)
                    elif op.sig:
                        ins.then_inc(op.sem, 1)
                stats[e] = (len(self.ops[e]), nw)

            @block.tensor
            def _(eng):
                run('pe', eng)

            @block.scalar
            def _(eng):
                run('act', eng)

            @block.vector
            def _(eng):
                run('dve', eng)

            @block.gpsimd
            def _(eng):
                run('pool', eng)

            @block.sync
            def _(eng):
                run('sp', eng)
            self.stats = stats


def _cvmap():
    m = {}
    c = 0
    for name, n in [('n1', 16), ('n2', 16), ('scw', 128), ('scb', 32), ('sd', 16), ('snw', 16),
                    ('lcw', 32), ('lcb', 8), ('lba', 8), ('lbx', 8), ('lam', 8),
                    ('mcw', 32), ('mcb', 8), ('mnw', 8), ('nf', 16)]:
        m[name] = c
        c += n
    return m, c


CVM, NCV = _cvmap()


class Ctx:
    pass


def build(nl=NL, dbg=None, mixers=('ssd', 'lru', 'ml'), do_ffn=True, mixtest=False):
    nc = bass.Bass("TRN2", target_bir_lowering=False)
    dbg = dbg or ()
    IN = lambda n, s, dt=F32: nc.dram_tensor(n, list(s), dt, kind="ExternalInput").ap()

    def SCR(n, s, dt=F32):
        kind = "ExternalOutput" if n in dbg else "Internal"
        if mixtest and n in ('zT', 'xbcT', 'dtT', 'lgT', 'lxT', 'mxT', 'moT'):
            kind = "ExternalInput"
        return nc.dram_tensor(n, list(s), dt, kind=kind).ap()
    x = IN("x", [T, D])
    if mixtest:
        w_in = w_out = w_gu = w_dn = None
    else:
        w_in = IN("w_in", [NL, D, DIN]); w_out = IN("w_out", [NL, 2 * D, D])
        w_gu = IN("w_gate_up", [NL, D, 2 * DFF]); w_dn = IN("w_down", [NL, DFF, D])
    cv_d = IN("cv", [NL, 128, NCV])
    hv_d = IN("hv", [NL, 32, 4])
    bdq_d = IN("bdq", [NL, 3, 8, 128, 128])
    lbd_d = IN("lbd", [NL, 2, 8, 128, 128])
    wif_d = IN("wif", [NL, 128, 24, 8])
    cst_d = IN("cst", [128, 128 + 512])
    c32_d = IN("c32", [128, 2048 + 4 + 8 * 128 + 16 * 128])
    nfb_d = IN("nfb", [1, D])
    out_d = nc.dram_tensor("out", [T, D], F32, kind="ExternalOutput").ap()
    hT = SCR("hT", [D, T])
    zT = SCR("zT", [2048, T]); xbcT = SCR("xbcT", [4096, T], BF16); dtT = SCR("dtT", [32, T])
    lgT = SCR("lgT", [1024, T]); lxT = SCR("lxT", [1024, T], BF16)
    mxT = SCR("mxT", [1024, T], BF16); moT = SCR("moT", [1024, T])
    yT = SCR("yT", [4096, T], BF16)
    mq_d = SCR("mqT", [1024, T], BF16); mk_d = SCR("mkT", [1024, T], BF16); mv_d = SCR("mvT", [1024, T], BF16)

    S = Sched(nc)
    with contextlib.ExitStack() as st:
        _tn = [0]

        def TL(shape, dt, name=None):
            _tn[0] += 1
            return st.enter_context(nc.sbuf_tensor(name or ("t%d" % _tn[0]), list(shape), dt))
        identf = TL([128, 128], F32); identb = TL([128, 128], BF16); mneg = TL([128, 512], BF16)
        onesf = TL([128, 128], F32)
        cvs = [TL([128, NCV], F32) for _ in range(NL)]
        B_const = Buf('const', const=True)
        AR = TL([128, 30720], F32, "arena")
        WS = [TL([128, 44 * 128], BF16) for _ in range(3)]
        WSB = [Buf('ws%d' % i) for i in range(3)]
        EV = [TL([128, 2048], F32) for _ in range(2)]
        EVB = [Buf('ev%d' % i) for i in range(2)]
        SM = [TL([128, 512], F32) for _ in range(6)]
        SMB = [Buf('sm%d' % i) for i in range(6)]
        RS = [TL([128, 512], F32) for _ in range(2)]
        RSB = [Buf('rs%d' % i) for i in range(2)]
        PS = [st.enter_context(nc.psum_tensor("ps%d" % i, [128, 512], F32)) for i in range(8)]
        PSB = [Buf('psb%d' % i, excl=True) for i in range(8)]
        B_h = Buf('hT'); B_proj = Buf('proj'); B_y = Buf('yT'); B_out = Buf('out')
        B_AR = Buf('arena')
        B_mq = Buf('mqkv')

        def arv(off, shape, dt):
            n = int(np.prod(shape[1:]))
            nb = n * (2 if dt == BF16 else 4)
            assert off % 4 == 0 and off + nb <= 122880, (off, nb)
            v = AR[0:shape[0], off // 4:(off + nb + 3) // 4]
            if dt == BF16:
                v = v.bitcast(BF16)
            if len(shape) == 3:
                v = v.rearrange("p (a b) -> p a b", b=shape[2])
            elif len(shape) == 4:
                v = v.rearrange("p (a b c) -> p a b c", b=shape[2], c=shape[3])
            return v

        def dma(eng, out, in_, rd, wr):
            return S.add(eng, lambda e: e.dma_start(out=out, in_=in_), rd, wr, dma=True)

        def mm(out, lhsT, rhs, start, stop, rd, wr):
            return S.add('pe', lambda e: e.matmul(out, lhsT=lhsT, rhs=rhs, start=start, stop=stop), rd, wr)

        def tr(out, in_, ident, rd, wr):
            return S.add('pe', lambda e: e.transpose(out, in_, ident), rd, wr)

        def act(out, in_, func, rd, wr, bias=None, scale=None):
            kw = {}
            if bias is not None:
                kw['bias'] = bias
            if scale is not None:
                kw['scale'] = scale
            return S.add('act', lambda e: e.activation(out=out, in_=in_, func=func, **kw), rd, wr)

        def tt(eng, out, a, b, op, rd, wr):
            return S.add(eng, lambda e: e.tensor_tensor(out=out, in0=a, in1=b, op=op), rd, wr)

        def ts(eng, out, a, s1, op0, rd, wr, s2=None, op1=None):
            if op1 is None:
                return S.add(eng, lambda e: e.tensor_scalar(out=out, in0=a, scalar1=s1, scalar2=None, op0=op0), rd, wr)
            return S.add(eng, lambda e: e.tensor_scalar(out=out, in0=a, scalar1=s1, scalar2=s2, op0=op0, op1=op1), rd, wr)

        def stt(out, in0, scalar, in1, op0, op1, rd, wr):
            return S.add('dve', lambda e: e.scalar_tensor_tensor(out=out, in0=in0, scalar=scalar, in1=in1, op0=op0, op1=op1), rd, wr)

        def cp(eng, out, in_, rd, wr):
            if eng == 'act':
                return S.add('act', lambda e: e.activation(out=out, in_=in_, func=AF.Copy), rd, wr)
            return S.add(eng, lambda e: e.tensor_copy(out=out, in_=in_), rd, wr)

        def memset(eng, ap, val, wr):
            return S.add(eng, lambda e: e.memset(ap, val), (), wr)

        def recip(out, in_, rd, wr):
            return S.add('dve', lambda e: e.reciprocal(out=out, in_=in_), rd, wr)

        rot = {}

        def nxt(key, n):
            rot[key] = (rot.get(key, -1) + 1) % n
            return rot[key]

        B_c = Buf('cinit')
        dma('sp', identf[:], cst_d[:, 0:128], [], [B_c])
        dma('pool', identb[:], cst_d[:, 0:128], [], [B_c])
        dma('pool', mneg[:], cst_d[:, 128:640], [], [B_c])
        for l in range(NL):
            dma('sp', cvs[l][:], cv_d[l], [], [B_c])
        memset('dve', onesf[:], 1.0, [B_c])
        S.barrier()

        def stage0():
            for tt_ in range(16):
                s = nxt('ev', 2)
                xt = EV[s]
                dma('sp', xt[:], x[tt_ * 128:(tt_ + 1) * 128, :], [], [EVB[s]])
                s2 = nxt('ev', 2)
                ho = EV[s2][:].rearrange("p (k t) -> p k t", t=128)
                for dg in range(4):
                    b = nxt('ps', 8)
                    for j in range(4):
                        k = dg * 4 + j
                        tr(PS[b][:, j * 128:(j + 1) * 128], xt[:, k * 128:(k + 1) * 128], identf[:], [EVB[s]], [PSB[b]])
                    cp('act' if dg % 2 == 0 else 'dve', EV[s2][:, dg * 512:(dg + 1) * 512], PS[b][:], [PSB[b]], [EVB[s2]])
                dma('sp', hT[:, tt_ * 128:(tt_ + 1) * 128].rearrange("(k p) t -> p k t", p=128), ho, [EVB[s2]], [B_h])

        def norm_stage(cvt, ncol, uT, B_u, tok0, ntok):
            for ti in range(ntok // 512):
                t0 = tok0 + ti * 512
                pb = nxt('ps', 8)
                for k in range(16):
                    s = nxt('sm', 6)
                    dma('sp', SM[s][:], hT[k * 128:(k + 1) * 128, t0:t0 + 512], [B_h], [SMB[s]])
                    s2 = nxt('sm', 6)
                    act(SM[s2][:], SM[s][:], AF.Square, [SMB[s]], [SMB[s2]])
                    mm(PS[pb][:], onesf[:], SM[s2][:], k == 0, k == 15, [SMB[s2]], [PSB[pb]])
                sr = nxt('rs', 2)
                act(RS[sr][:], PS[pb][:], AF.Sqrt, [PSB[pb]], [RSB[sr]], bias=EPS, scale=1.0 / D)
                recip(RS[sr][:], RS[sr][:], [RSB[sr]], [RSB[sr]])
                for k in range(16):
                    s = nxt('sm', 6)
                    dma('sp', SM[s][:], hT[k * 128:(k + 1) * 128, t0:t0 + 512], [B_h], [SMB[s]])
                    stt(uT[:, k, ti * 512:(ti + 1) * 512], SM[s][:], cvt[:, ncol + k:ncol + k + 1], RS[sr][:],
                        ALU.mult, ALU.mult, [SMB[s], RSB[sr]], [B_u])

        def load_w(src, nk, width):
            s = nxt('ws', 3)
            wv = WS[s][:, 0:nk * width].rearrange("p (k m) -> p k m", m=width)
            dma('pool', wv, src.rearrange("(k p) m -> p k m", p=128), [], [WSB[s]])
            return wv, WSB[s]

        def proj_stage(l, uT, B_u):
            segs = [(0, 2048, zT, F32), (2048, 4096, xbcT, BF16), (6144, 32, dtT, F32), (6176, 1024, lgT, F32),
                    (7200, 1024, lxT, BF16), (8224, 1024, mxT, BF16), (9248, 1024, moT, F32)]
            ecnt = 0
            for (c0, n, dst, dt_) in segs:
                wwid = 256 if n >= 256 else n
                for wb in range(n // wwid):
                    wv, wB = load_w(w_in[l][:, c0 + wb * wwid:c0 + (wb + 1) * wwid], 16, wwid)
                    for sb in range(max(1, wwid // 128)):
                        m = min(128, wwid)
                        es = nxt('ev', 2)
                        if dt_ == BF16:
                            evv = EV[es][:].bitcast(BF16)[:, 0:2048]
                        else:
                            evv = EV[es][:]
                        for ti in range(4):
                            pb = nxt('ps', 4)
                            for k in range(16):
                                mm(PS[pb][0:m, :], wv[:, k, sb * 128:sb * 128 + m], uT[:, k, ti * 512:(ti + 1) * 512],
                                   k == 0, k == 15, [wB, B_u], [PSB[pb]])
                            ecnt += 1
                            cp('act' if ecnt % 2 else 'dve', evv[0:m, ti * 512:(ti + 1) * 512], PS[pb][0:m, :], [PSB[pb]], [EVB[es]])
                        r0 = wb * wwid + sb * 128
                        dma('sp', dst[r0:r0 + m, :], evv[0:m, :], [EVB[es]], [B_proj])

        def resid_dense(wsrc, nk, rhsT, B_rhs, tok0, ntok):
            for db in range(16):
                wv, wB = load_w(wsrc[:, db * 128:(db + 1) * 128], nk, 128)
                for ti in range(ntok // 512):
                    t0 = tok0 + ti * 512
                    pb = nxt('ps', 4)
                    s = nxt('sm', 6)
                    dma('sp', SM[s][:], hT[db * 128:(db + 1) * 128, t0:t0 + 512], [B_h], [SMB[s]])
                    for k in range(nk):
                        mm(PS[pb][:], wv[:, k, :], rhsT[:, k, ti * 512:(ti + 1) * 512], k == 0, k == nk - 1,
                           [wB, B_rhs], [PSB[pb]])
                    tt('dve', SM[s][:], SM[s][:], PS[pb][:], ALU.add, [SMB[s], PSB[pb]], [SMB[s]])
                    dma('act', hT[db * 128:(db + 1) * 128, t0:t0 + 512], SM[s][:], [SMB[s]], [B_h])

        def wout_stage(l):
            for half in range(2):
                yv = arv(0, [128, 32, 1024], BF16)
                B_yv = Buf('yv')
                for q in range(4):
                    dma('sp', yv[:, q * 8:(q + 1) * 8, :],
                        yT[q * 1024:(q + 1) * 1024, half * 1024:(half + 1) * 1024].rearrange("(k p) t -> p k t", p=128),
                        [B_y], [B_yv])
                resid_dense(w_out[l], 32, yv, B_yv, half * 1024, 1024)
                S.barrier()

        def ffn_stage(l):
            for half in range(2):
                u2 = arv(0, [128, 16, 1024], BF16)
                aT = arv(32768, [128, 44, 1024], BF16)
                B_u2 = Buf('u2'); B_a = Buf('aT')
                norm_stage(cvs[l], CVM['n2'], u2, B_u2, half * 1024, 1024)
                for fb in range(44):
                    s = nxt('ws', 3)
                    wv = WS[s][:, 0:16 * 256].rearrange("p (k m) -> p k m", m=256)
                    dma('pool', wv[:, :, 0:128], w_gu[l][:, fb * 128:(fb + 1) * 128].rearrange("(k p) m -> p k m", p=128), [], [WSB[s]])
                    dma('pool', wv[:, :, 128:256], w_gu[l][:, DFF + fb * 128:DFF + (fb + 1) * 128].rearrange("(k p) m -> p k m", p=128), [], [WSB[s]])
                    for ti in range(2):
                        pg = nxt('ps', 4)
                        for k in range(16):
                            mm(PS[pg][:], wv[:, k, 0:128], u2[:, k, ti * 512:(ti + 1) * 512], k == 0, k == 15, [WSB[s], B_u2], [PSB[pg]])
                        pu = nxt('ps', 4)
                        for k in range(16):
                            mm(PS[pu][:], wv[:, k, 128:256], u2[:, k, ti * 512:(ti + 1) * 512], k == 0, k == 15, [WSB[s], B_u2], [PSB[pu]])
                        sg = nxt('sm', 6)
                        act(SM[sg][:], PS[pg][:], AF.Silu, [PSB[pg]], [SMB[sg]])
                        tt('dve', aT[:, fb, ti * 512:(ti + 1) * 512], SM[sg][:], PS[pu][:], ALU.mult, [SMB[sg], PSB[pu]], [B_a])
                resid_dense(w_dn[l], 44, aT, B_a, half * 1024, 1024)
                S.barrier()

        def final_stage():
            wB_ = arv(0, [128, 2048], F32)
            B_w = Buf('nfw')
            dma('sp', wB_, nfb_d[0:1, :].partition_broadcast(128), [], [B_w])
            ssq = arv(8192, [128, 16], F32)
            B_s = Buf('ssq')
            hins = [arv(16384 + i * 8192, [128, 16, 128], F32) for i in range(2)]
            htoks = [arv(32768 + i * 8192, [128, 2048], F32) for i in range(2)]
            junk = arv(49152, [128, 2048], F32)
            B_hin = [Buf('hin%d' % i) for i in range(2)]
            B_ht = [Buf('ht%d' % i) for i in range(2)]
            B_j = Buf('junk')
            for tt_ in range(16):
                s = tt_ % 2
                hin = hins[s]
                ht = htoks[s]
                dma('sp', hin, hT[:, tt_ * 128:(tt_ + 1) * 128].rearrange("(k p) t -> p k t", p=128), [B_h], [B_hin[s]])
                for dg in range(4):
                    b = nxt('ps', 8)
                    for j in range(4):
                        k = dg * 4 + j
                        tr(PS[b][:, j * 128:(j + 1) * 128], hin[:, k, :], identf[:], [B_hin[s]], [PSB[b]])
                    cp('act' if dg % 2 == 0 else 'dve', ht[:, dg * 512:(dg + 1) * 512], PS[b][:], [PSB[b]], [B_ht[s]])
                sq = ssq[:, tt_:tt_ + 1]
                S.add('act', (lambda o, i, a_: (lambda e: e.activation(out=o, in_=i, func=AF.Square, accum_out=a_)))(junk, ht, sq),
                      [B_ht[s]], [B_j, B_s])
                act(sq, sq, AF.Sqrt, [B_s], [B_s], bias=EPS, scale=1.0 / D)
                recip(sq, sq, [B_s], [B_s])
                stt(ht, ht, sq, wB_, ALU.mult, ALU.mult, [B_ht[s], B_s, B_w], [B_ht[s]])
                dma('sp', out_d[tt_ * 128:(tt_ + 1) * 128, :], ht, [B_ht[s]], [B_out])

        def mixer_stage(l):
            if not mixers:
                zt = arv(0, [128, 2048], BF16)
                B_z = Buf('zt')
                memset('dve', zt, 0.0, [B_z])
                for q in range(32):
                    dma('sp', yT[q * 128:(q + 1) * 128, :], zt, [B_z], [B_y])
                return
            cvt = cvs[l]
            aoff = [0]

            def NT(shape, dt, name):
                n = int(np.prod(shape[1:])) * (2 if dt == BF16 else 4)
                n = (n + 31) // 32 * 32
                v = arv(aoff[0], shape, dt)
                aoff[0] += n
                return v, Buf(name)

            def areset(o=0):
                aoff[0] = o
            c32, B_c32 = NT([128, 5124], F32, 'c32')
            hvt, B_hv = NT([32, 4], F32, 'hvt')
            dma('sp', c32, c32_d, [], [B_c32])
            dma('sp', hvt, hv_d[l], [], [B_hv])
            rmask = c32[:, 0:2048]
            dmask4 = c32[:, 2048:2052]
            gsel = c32[:, 2052:2052 + 1024].rearrange("p (g s) -> p g s", s=128)
            hexp = c32[:, 3076:3076 + 2048].rearrange("p (b c) -> p b c", c=128)
            xr = [NT([128, 2052], BF16, 'xr%d' % i) for i in range(2)]
            dgs = [[NT([128, 128], BF16, 'dg%d_%d' % (i, k)) for k in range(4)] for i in range(2)]
            for i in range(2):
                memset('pool', xr[i][0][:, 0:4], 0.0, [xr[i][1]])
            base_off = aoff[0]

            def conv_block(src, wname, nb, blk, evac, keep_raw=None):
                s = nxt('xr', 2)
                xrt, xb = xr[s]
                dma('sp', xrt[:, 3:2051], src, [B_proj], [xb])
                dg = dgs[s]
                for k in range(4):
                    col = CVM[wname] + k * nb + blk
                    ts('pool', dg[k][0], identf[:], cvt[:, col:col + 1], ALU.mult, [B_c], [dg[k][1]])
                for ti in range(4):
                    pb = 5 + nxt('psm', 3)
                    for k in range(4):
                        mm(PS[pb][:], dg[k][0], xrt[:, ti * 512 + k:ti * 512 + k + 512], k == 0, k == 3,
                           [dg[k][1], xb], [PSB[pb]])
                    evac(ti, pb)
                return xrt, xb

            def lru():
                areset(base_off)
                c1, B_c1 = NT([128, 8], F32, 'c1')
                lam = cvt[:, CVM['lam']:CVM['lam'] + 8]
                act(c1, lam, AF.Exp, [B_c], [B_c1], scale=-1.0)
                act(c1, c1, AF.Ln, [B_c1], [B_c1], bias=1.0)
                ts('dve', c1, c1, -8.0, ALU.mult, [B_c1], [B_c1])
                wab = [NT([128, 128], BF16, 'wab%d' % i) for i in range(2)]
                wxb = [NT([128, 128], BF16, 'wxb%d' % i) for i in range(2)]
                xc32, B_xc = NT([128, 2048], F32, 'xc32')
                xcb, B_xcb = NT([128, 2048], BF16, 'xcb')
                r_, B_r = NT([128, 2048], F32, 'r')
                i_, B_i = NT([128, 2048], F32, 'i')
                a_, B_a = NT([128, 2048], F32, 'a')
                hs, B_hs = NT([128, 2048], F32, 'hs')
                lg, B_lg = NT([128, 2048], F32, 'lg')
                tmp, B_tmp = NT([128, 2048], F32, 'tmp')
                yo, B_yo = NT([128, 2048], BF16, 'yo')
                for b in range(8):
                    w = b % 2
                    dma('pool', wab[w][0], lbd_d[l, 0, b], [], [wab[w][1]])
                    dma('pool', wxb[w][0], lbd_d[l, 1, b], [], [wxb[w][1]])
                    bcol = CVM['lcb'] + b

                    def ev(ti, pb, bcol=bcol):
                        act(xc32[:, ti * 512:(ti + 1) * 512], PS[pb][:], AF.Identity, [PSB[pb]], [B_xc], bias=cvt[:, bcol:bcol + 1])
                    conv_block(lxT[b * 128:(b + 1) * 128, :], 'lcw', 8, b, ev)
                    if LSTOP == 1:
                        dma('sp', yT[2048 + b * 128:2048 + (b + 1) * 128, :], xr[0][0][:, 0:2048], [B_xc], [B_y])
                        continue
                    cp('dve', xcb, xc32, [B_xc], [B_xcb])
                    for ti in range(4):
                        sl = slice(ti * 512, (ti + 1) * 512)
                        pb = 5 + nxt('psm', 3)
                        mm(PS[pb][:], wab[w][0], xcb[:, sl], True, True, [wab[w][1], B_xcb], [PSB[pb]])
                        act(r_[:, sl], PS[pb][:], AF.Sigmoid, [PSB[pb]], [B_r], bias=cvt[:, CVM['lba'] + b:CVM['lba'] + b + 1])
                        pb = 5 + nxt('psm', 3)
                        mm(PS[pb][:], wxb[w][0], xcb[:, sl], True, True, [wxb[w][1], B_xcb], [PSB[pb]])
                        act(i_[:, sl], PS[pb][:], AF.Sigmoid, [PSB[pb]], [B_i], bias=cvt[:, CVM['lbx'] + b:CVM['lbx'] + b + 1])
                    if LSTOP == 2:
                        dma('sp', yT[2048 + b * 128:2048 + (b + 1) * 128, :], xcb, [B_r, B_i], [B_y])
                        continue
                    ts('dve', r_, r_, c1[:, b:b + 1], ALU.mult, [B_r, B_c1], [B_r])
                    act(a_, r_, AF.Exp, [B_r], [B_a])
                    act(r_, r_, AF.Exp, [B_r], [B_r], scale=2.0)
                    act(r_, r_, AF.Sqrt, [B_r], [B_r], scale=-1.0, bias=1.0)
                    tt('dve', i_, i_, xc32, ALU.mult, [B_i, B_xc], [B_i])
                    tt('dve', i_, i_, r_, ALU.mult, [B_i, B_r], [B_i])
                    if LSTOP == 3:
                        dma('sp', yT[2048 + b * 128:2048 + (b + 1) * 128, :], xcb, [B_a, B_i], [B_y])
                        continue
                    S.add('dve', lambda e: e.tensor_tensor_scan(out=hs, data0=a_, data1=i_, initial=0.0, op0=ALU.mult, op1=ALU.add),
                          [B_a, B_i], [B_hs])
                    dma('sp', lg, lgT[b * 128:(b + 1) * 128, :], [B_proj], [B_lg])
                    act(tmp, lg, AF.Square, [B_lg], [B_tmp])
                    ts('dve', tmp, tmp, 0.044715, ALU.mult, [B_tmp], [B_tmp], s2=1.0, op1=ALU.add)
                    tt('dve', tmp, tmp, lg, ALU.mult, [B_tmp, B_lg], [B_tmp])
                    act(tmp, tmp, AF.Sigmoid, [B_tmp], [B_tmp], scale=1.5957691216057308)
                    tt('dve', tmp, tmp, lg, ALU.mult, [B_tmp, B_lg], [B_tmp])
                    tt('dve', yo, hs, tmp, ALU.mult, [B_hs, B_tmp], [B_yo])
                    dma('sp', yT[2048 + b * 128:2048 + (b + 1) * 128, :], yo, [B_yo], [B_y])

            def ssd():
                areset(base_off)
                dtrF, B_dt = NT([128, 2048], F32, 'dtr')
                csTF, B_cs = NT([128, 2048], F32, 'csT')
                tmp32, B_t32 = NT([32, 2048], F32, 'tmp32')
                memset('dve', dtrF, 0.0, [B_dt])
                memset('dve', csTF, 0.0, [B_cs])
                dtr = dtrF[0:32, :]
                csT = csTF[0:32, :]
                rmask32 = rmask[0:32, :]
                aneg, B_an = NT([32, 1], F32, 'aneg')
                dt_tok, B_dtt = NT([128, 16, 32], F32, 'dt_tok')
                cs_tok, B_cst = NT([128, 16, 32], F32, 'cs_tok')
                ncs_tok, B_ncs = NT([128, 16, 32], F32, 'ncs_tok')
                w2_tok, B_w2 = NT([128, 16, 32], F32, 'w2_tok')
                te_tok, B_te = NT([128, 16, 32], F32, 'te_tok')
                cdB, B_cd = NT([128, 16, 32], F32, 'cdB')
                R16F, B_R16 = NT([128, 16, 32], F32, 'R16')
                memset('dve', R16F, 0.0, [B_R16])
                R16 = R16F[0:32]
                dma('sp', dtr, dtT, [B_proj], [B_dt])
                if SSTOP == 0.1:
                    return
                act(tmp32, dtr, AF.Exp, [B_dt, B_hv], [B_t32], bias=hvt[:, 0:1])
                act(dtr, tmp32, AF.Ln, [B_t32], [B_dt], bias=1.0)
                act(aneg, hvt[:, 1:2], AF.Exp, [B_hv], [B_an])
                ts('dve', aneg, aneg, -1.0, ALU.mult, [B_an], [B_an])
                ts('dve', tmp32, dtr, aneg[:, 0:1], ALU.mult, [B_dt, B_an], [B_t32])
                if SSTOP == 0.2:
                    return
                S.add('dve', lambda e: e.tensor_tensor_scan(out=csT, data0=rmask32, data1=tmp32, initial=0.0, op0=ALU.mult, op1=ALU.add),
                      [B_c32, B_t32], [B_cs])
                if SSTOP == 0.3:
                    return
                for c in range(16):
                    mm(PS[5][:, c * 32:(c + 1) * 32], dtrF[:, c * 128:(c + 1) * 128], identf[:, 0:32], True, True, [B_dt], [PSB[5]])
                    mm(PS[6][:, c * 32:(c + 1) * 32], csTF[:, c * 128:(c + 1) * 128], identf[:, 0:32], True, True, [B_cs], [PSB[6]])
                cp('act', dt_tok.rearrange("p c h -> p (c h)"), PS[5][:], [PSB[5]], [B_dtt])
                cp('dve', cs_tok.rearrange("p c h -> p (c h)"), PS[6][:], [PSB[6]], [B_cst])
                if SSTOP == 0.4:
                    return
                csend = csT.rearrange("p (c l) -> p c l", l=128)[:, :, 127]
                tt('dve', R16, csend.unsqueeze(2).to_broadcast([32, 16, 32]),
                   identf[0:32, 0:32].unsqueeze(1).to_broadcast([32, 16, 32]), ALU.mult, [B_cs, B_c], [B_R16])
                mm(PS[7][:], onesf[:], R16F.rearrange("p c h -> p (c h)"), True, True, [B_R16], [PSB[7]])
                act(cdB.rearrange("p c h -> p (c h)"), PS[7][:], AF.Exp, [PSB[7]], [B_cd])
                if SSTOP == 0.5:
                    return
                tt('dve', te_tok.rearrange("p c h -> p (c h)"), PS[7][:], cs_tok.rearrange("p c h -> p (c h)"), ALU.subtract,
                   [PSB[7], B_cst, B_cd], [B_te])
                if SSTOP == 0.6:
                    return
                act(te_tok, te_tok, AF.Exp, [B_te], [B_te])
                if SSTOP == 0.7:
                    return
                tt('dve', w2_tok, dt_tok, te_tok, ALU.mult, [B_dtt, B_te], [B_w2])
                ts('dve', ncs_tok, cs_tok, -1.0, ALU.mult, [B_cst], [B_ncs])
                if SSTOP == 1:
                    return
                xs32 = [(EV[i][:], EVB[i]) for i in range(2)]
                BT, B_BT = WS[0][:, 0:2048], Buf('BT')
                CT, B_CT = WS[0][:, 2048:4096], Buf('CT')
                Btok, B_Btok = NT([128, 16, 128], BF16, 'Btok')
                xd, B_xd = WS[1][:, 0:4096].rearrange('p (c x) -> p c x', x=256), Buf('xd')
                xdte, B_xdte = WS[2][:, 0:4096].rearrange('p (c x) -> p c x', x=256), Buf('xdte')
                sel, B_sel = NT([128, 32, 128], F32, 'sel')
                cp('dve', sel, identf[:, 0:32].unsqueeze(2).to_broadcast([128, 32, 128]), [B_c], [B_sel])
                dec = [(SM[i][:].rearrange('p (j t) -> p j t', t=128), SMB[i]) for i in range(2)]
                MT = [NT([128, 4, 128], BF16, 'MT%d' % i) for i in range(2)]
                Hf, B_Hf = NT([128, 4, 64], F32, 'Hf')
                Hb, B_Hb = NT([128, 256], BF16, 'Hb')
                Ht, B_Ht = NT([128, 4, 64], F32, 'Ht')
                ecs, B_ecs = SM[2][:].rearrange('p (b t) -> p b t', t=256), SMB[2]
                t1, B_t1 = SM[3][:].rearrange('p (b t) -> p b t', t=256), SMB[3]
                zt, B_zt = SM[4][:].rearrange('p (b t) -> p b t', t=256), SMB[4]
                sq, B_sq = SM[5][:].rearrange('p (b t) -> p b t', t=256), SMB[5]
                rstd, B_rstd = RS[0][:, 0:256], RSB[0]
                yo2, B_yo2 = NT([128, 2, 256], BF16, 'yo2')
                B_sc = [PSB[0], PSB[7]]
                B_st = PSB[6]
                PSbf7 = PS[7][:].bitcast(BF16)
                for g in range(8):
                    for bl in range(2):
                        blk = 2 * g + bl
                        xst, xsb = xs32[bl]

                        def ev(ti, pb, xst=xst, xsb=xsb, blk=blk):
                            act(xst[:, ti * 512:(ti + 1) * 512], PS[pb][:], AF.Silu, [PSB[pb]], [xsb],
                                bias=cvt[:, CVM['scb'] + blk:CVM['scb'] + blk + 1])
                        conv_block(xbcT[blk * 128:(blk + 1) * 128, :], 'scw', 32, blk, ev)
                        for c4 in range(4):
                            pb = 5 + nxt('psm', 3)
                            for j in range(4):
                                c = c4 * 4 + j
                                tr(PS[pb][:, j * 128:(j + 1) * 128], xst[:, c * 128:(c + 1) * 128], identf[:], [xsb], [PSB[pb]])
                            pin = PS[pb][:].rearrange("p (c h x) -> p c h x", h=2, x=64)
                            for (dst, B_dst, wt, B_wt) in ((xd, B_xd, dt_tok, B_dtt), (xdte, B_xdte, w2_tok, B_w2)):
                                o = dst[:, c4 * 4:c4 * 4 + 4, bl * 128:(bl + 1) * 128].rearrange("p c (h x) -> p c h x", x=64)
                                wv_ = wt[:, c4 * 4:c4 * 4 + 4, 2 * blk:2 * blk + 2].unsqueeze(3).to_broadcast([128, 4, 2, 64])
                                tt('dve', o, pin, wv_, ALU.mult, [PSB[pb], B_wt], [B_dst])
                    for (dstT, B_dstT, blk) in ((BT, B_BT, 16 + g), (CT, B_CT, 24 + g)):
                        def ev(ti, pb, dstT=dstT, B_dstT=B_dstT, blk=blk):
                            act(dstT[:, ti * 512:(ti + 1) * 512], PS[pb][:], AF.Silu, [PSB[pb]], [B_dstT],
                                bias=cvt[:, CVM['scb'] + blk:CVM['scb'] + blk + 1])
                        conv_block(xbcT[blk * 128:(blk + 1) * 128, :], 'scw', 32, blk, ev)
                    for c8 in range(2):
                        for j in range(8):
                            c = c8 * 8 + j
                            tr(PSbf7[:, j * 128:(j + 1) * 128], BT[:, c * 128:(c + 1) * 128], identb[:], [B_BT], [PSB[7]])
                        cp('act', Btok[:, c8 * 8:(c8 + 1) * 8, :].rearrange("p c n -> p (c n)"), PSbf7[:, 0:1024], [PSB[7]], [B_Btok])
                    if SSTOP == 2:
                        return
                    memset('dve', Hf, 0.0, [B_Hf])
                    memset('dve', Hb, 0.0, [B_Hb])

                    def partA(c):
                        p = c % 2
                        sc = PS[0][:, 0:128] if p == 0 else PS[7][:, 0:128]
                        mm(sc, BT[:, c * 128:(c + 1) * 128], CT[:, c * 128:(c + 1) * 128], True, True, [B_BT, B_CT], [B_sc[p]])
                        pd = 1 + p
                        mm(PS[pd][:], identb[:], mneg[:], True, False, [B_c], [PSB[pd]])
                        for j in range(4):
                            mm(PS[pd][:, j * 128:(j + 1) * 128], sel[:, 4 * g + j, :], csTF[:, c * 128:(c + 1) * 128], False, j == 3,
                               [B_sel, B_cs], [PSB[pd]])

                    def chain(c):
                        p = c % 2
                        sc = PS[0][:, 0:128] if p == 0 else PS[7][:, 0:128]
                        d_, B_d = dec[p]
                        m_, B_m = MT[p]
                        pd = 1 + p
                        for j in range(4):
                            h = 4 * g + j
                            act(d_[:, j, :], PS[pd][:, j * 128:(j + 1) * 128], AF.Exp, [PSB[pd], B_ncs], [B_d],
                                bias=ncs_tok[:, c, h:h + 1])
                        tt('dve', m_, d_, sc.unsqueeze(1).to_broadcast([128, 4, 128]), ALU.mult, [B_d, B_sc[p]], [B_m])

                    def partB(c):
                        p = c % 2
                        cc = c % 2
                        m_, B_m = MT[p]
                        for j in range(4):
                            po = (j % 2) * 64
                            col = (j // 2) * 256 + cc * 128
                            mm(PS[3][po:po + 64, col:col + 128], xd[:, c, j * 64:(j + 1) * 64], m_[:, j, :], True, True,
                               [B_xd, B_m], [PSB[3]])
                        for j in range(4):
                            po = (j % 2) * 64
                            col = (j // 2) * 256 + cc * 128
                            mm(PS[4][po:po + 64, col:col + 128], Hb[:, j * 64:(j + 1) * 64], CT[:, c * 128:(c + 1) * 128], True, True,
                               [B_Hb, B_CT], [PSB[4]])
                        mm(PS[6][:, 0:256], Btok[:, c, :], xdte[:, c, :], True, True, [B_Btok, B_xdte], [B_st])

                    def state_update(c):
                        tt('dve', Ht, Hf, cdB[:, c, 4 * g:4 * g + 4].unsqueeze(2).to_broadcast([128, 4, 64]), ALU.mult,
                           [B_Hf, B_cd], [B_Ht])
                        tt('dve', Hf, Ht, PS[6][:, 0:256].rearrange("p (j x) -> p j x", x=64), ALU.add, [B_Ht, B_st], [B_Hf])
                        cp('act', Hb, Hf.rearrange("p j x -> p (j x)"), [B_Hf], [B_Hb])

                    t1s = [(t1, B_t1), (RS[1][:].rearrange('p (b t) -> p b t', t=256), RSB[1])]

                    def combine1(c):
                        t0 = (c - 1) * 128
                        t1, B_t1 = t1s[(c // 2) % 2]
                        for bl in range(2):
                            mm(PS[5][:, bl * 256:(bl + 1) * 256], hexp[:, 2 * g + bl, :], csTF[:, t0:t0 + 256], True, True,
                               [B_c32, B_cs], [PSB[5]])
                        act(ecs.rearrange("p b t -> p (b t)"), PS[5][:], AF.Exp, [PSB[5]], [B_ecs])
                        tt('dve', t1.rearrange("p b t -> p (b t)"), PS[4][:], ecs.rearrange("p b t -> p (b t)"), ALU.mult,
                           [PSB[4], B_ecs], [B_t1])
                        tt('dve', t1.rearrange("p b t -> p (b t)"), t1.rearrange("p b t -> p (b t)"), PS[3][:], ALU.add,
                           [B_t1, PSB[3]], [B_t1])

                    def combine2(c):
                        t0 = (c - 1) * 128
                        t1, B_t1 = t1s[(c // 2) % 2]
                        for bl in range(2):
                            blk = 2 * g + bl
                            stt(t1[:, bl, :], xs32[bl][0][:, t0:t0 + 256], cvt[:, CVM['sd'] + blk:CVM['sd'] + blk + 1], t1[:, bl, :],
                                ALU.mult, ALU.add, [xs32[bl][1], B_t1], [B_t1])
                        dma('sp', zt, zT[2 * g * 128:(2 * g + 2) * 128, t0:t0 + 256].rearrange("(b p) t -> p b t", p=128), [B_proj], [B_zt])
                        act(zt, zt, AF.Silu, [B_zt], [B_zt])
                        tt('dve', t1, t1, zt, ALU.mult, [B_t1, B_zt], [B_t1])
                        tt('dve', sq, t1, t1, ALU.mult, [B_t1], [B_sq])
                        mm(PS[5][:, 0:256], onesf[:], sq[:, 0, :], True, False, [B_sq], [PSB[5]])
                        mm(PS[5][:, 0:256], onesf[:], sq[:, 1, :], False, True, [B_sq], [PSB[5]])
                        act(rstd, PS[5][:, 0:256], AF.Ln, [PSB[5]], [B_rstd], scale=1.0 / 256, bias=EPS)
                        act(rstd, rstd, AF.Exp, [B_rstd], [B_rstd], scale=-0.5)
                        for bl in range(2):
                            blk = 2 * g + bl
                            stt(yo2[:, bl, :], t1[:, bl, :], cvt[:, CVM['snw'] + blk:CVM['snw'] + blk + 1], rstd, ALU.mult, ALU.mult,
                                [B_t1, B_rstd], [B_yo2])
                        dma('sp', yT[2 * g * 128:(2 * g + 2) * 128, t0:t0 + 256].rearrange("(b p) t -> p b t", p=128), yo2, [B_yo2], [B_y])

                    partA(0)
                    chain(0)
                    for c in range(16):
                        if c + 1 < 16:
                            partA(c + 1)
                        partB(c)
                        if c + 1 < 16:
                            chain(c + 1)
                        state_update(c)
                        if c % 2 == 1:
                            combine1(c)
                        if c % 2 == 0 and c >= 2:
                            combine2(c - 1)
                    combine2(15)
                    if SSTOP == 6:
                        return

            def mlstm():
                areset(base_off)
                wifb, B_wif = NT([128, 24, 8], BF16, 'wifb')
                dma('pool', wifb, wif_d[l], [], [B_wif])
                bd = [[NT([128, 128], BF16, 'bd%d_%d' % (i, s_)) for s_ in range(2)] for i in range(3)]
                G = [NT([128, 2048], F32, 'G%d' % i) for i in range(4)]
                for i in range(4):
                    memset('dve', G[i][0], 0.0, [G[i][1]])
                q_sb, B_q = WS[0][:, 0:2048], Buf('q_sb')
                qs_sb, B_qs = WS[0][:, 2048:4096], Buf('qs_sb')
                k_sb, B_k = WS[1][:, 0:2048], Buf('k_sb')
                v_sb, B_v = WS[1][:, 2048:4096], Buf('v_sb')
                mc, B_mc = WS[2][:, 0:2048], Buf('mc')
                for b in range(8):
                    s_ = b % 2
                    for i in range(3):
                        dma('pool', bd[i][s_][0], bdq_d[l, i, b], [], [bd[i][s_][1]])

                    def ev(ti, pb, b=b):
                        act(mc[:, ti * 512:(ti + 1) * 512], PS[pb][:], AF.Silu, [PSB[pb]], [B_mc],
                            bias=cvt[:, CVM['mcb'] + b:CVM['mcb'] + b + 1])
                    xrt, xb = conv_block(mxT[b * 128:(b + 1) * 128, :], 'mcw', 8, b, ev)
                    for ti in range(4):
                        sl = slice(ti * 512, (ti + 1) * 512)
                        pb = 5 + nxt('psm', 3)
                        mm(PS[pb][:], bd[0][s_][0], mc[:, sl], True, True, [bd[0][s_][1], B_mc], [PSB[pb]])
                        act(q_sb[:, sl], PS[pb][:], AF.Copy, [PSB[pb]], [B_q])
                        act(qs_sb[:, sl], PS[pb][:], AF.Copy, [PSB[pb]], [B_qs], scale=1.0 / 16)
                        pb = 5 + nxt('psm', 3)
                        mm(PS[pb][:], bd[1][s_][0], mc[:, sl], True, True, [bd[1][s_][1], B_mc], [PSB[pb]])
                        cp('dve', k_sb[:, sl], PS[pb][:], [PSB[pb]], [B_k])
                        pb = 5 + nxt('psm', 3)
                        mm(PS[pb][:], bd[2][s_][0], xrt[:, 3 + ti * 512:3 + (ti + 1) * 512], True, True, [bd[2][s_][1], xb], [PSB[pb]])
                        cp('dve', v_sb[:, sl], PS[pb][:], [PSB[pb]], [B_v])
                        for i, (src, B_src) in enumerate(((q_sb, B_q), (k_sb, B_k), (v_sb, B_v))):
                            mm(PS[ti][0:8, :], wifb[:, i * 8 + b, :], src[:, sl], (b == 0 and i == 0), (b == 7 and i == 2),
                               [B_wif, B_src], [PSB[ti]])
                    dma('sp', mq_d[b * 128:(b + 1) * 128, :], qs_sb, [B_qs], [B_mq])
                    dma('sp', mk_d[b * 128:(b + 1) * 128, :], k_sb, [B_k], [B_mq])
                    dma('sp', mv_d[b * 128:(b + 1) * 128, :], v_sb, [B_v], [B_mq])
                G0, B_G0 = G[0]; G1, B_G1 = G[1]; G2, B_G2 = G[2]; G3, B_G3 = G[3]
                for ti in range(4):
                    cp('act', G0[0:8, ti * 512:(ti + 1) * 512], PS[ti][0:8, :], [PSB[ti]], [B_G0])
                sm_, B_sm = NT([4, 160], F32, 'mlsmall')
                nbf = sm_[:, 0:1]; gmax = sm_[:, 16:32]; mloc = sm_[:, 32:48]; mm_ = sm_[:, 48:65]
                sold = sm_[:, 80:96]; sloc = sm_[:, 96:112]; tmp16 = sm_[:, 112:128]
                ts('dve', nbf, hvt[0:4, 3:4], -1.0, ALU.mult, [B_hv], [B_sm])
                for ti in range(4):
                    sl = slice(ti * 512, (ti + 1) * 512)
                    pb = 5 + nxt('psm', 3)
                    mm(PS[pb][0:4, :], identf[:, 4:8], G0[:, sl], True, True, [B_G0], [PSB[pb]])
                    act(G1[0:4, sl], PS[pb][0:4, :], AF.Exp, [PSB[pb], B_sm], [B_G1], scale=-1.0, bias=nbf)
                act(G1[0:4, :], G1[0:4, :], AF.Ln, [B_G1], [B_G1], bias=1.0)
                ts('dve', G1[0:4, :], G1[0:4, :], -1.0, ALU.mult, [B_G1], [B_G1])
                S.add('dve', lambda e: e.tensor_tensor_scan(out=G2[0:4, :], data0=rmask[0:4, :], data1=G1[0:4, :], initial=0.0,
                                                            op0=ALU.mult, op1=ALU.add), [B_c32, B_G1], [B_G2])
                ts('dve', G0[0:4, :], G0[0:4, :], hvt[0:4, 2:3], ALU.add, [B_G0, B_hv], [B_G0])
                tt('dve', G1[0:4, :], G0[0:4, :], G2[0:4, :], ALU.subtract, [B_G0, B_G2], [B_G1])
                g3 = G1[0:4, :].rearrange("p (c l) -> p c l", l=128)
                S.add('dve', lambda e: e.tensor_reduce(out=gmax, in_=g3, axis=AX.X, op=ALU.max), [B_G1], [B_sm])
                gtot = G2[0:4, :].rearrange("p (c l) -> p c l", l=128)[:, :, 127]
                tt('dve', mloc, gtot, gmax, ALU.add, [B_G2, B_sm], [B_sm])
                memset('dve', mm_[:, 0:1], 0.0, [B_sm])
                for c in range(16):
                    stt(mm_[:, c + 1:c + 2], gtot[:, c:c + 1], mm_[:, c:c + 1], mloc[:, c:c + 1], ALU.add, ALU.max,
                        [B_G2, B_sm], [B_sm])
                for c in range(16):
                    S.add('dve', (lambda c: (lambda e: e.tensor_tensor_scan(out=G0[0:4, c * 128:(c + 1) * 128], data0=onesf[0:4, :],
                                                                            data1=G1[0:4, c * 128:(c + 1) * 128], initial=mm_[:, c:c + 1],
                                                                            op0=ALU.mult, op1=ALU.max)))(c), [B_G1, B_sm], [B_G0])
                tt('dve', tmp16, gtot, mm_[:, 0:16], ALU.add, [B_G2, B_sm], [B_sm])
                tt('dve', tmp16, tmp16, mm_[:, 1:17], ALU.subtract, [B_sm], [B_sm])
                act(sold, tmp16, AF.Exp, [B_sm], [B_sm])
                tt('dve', tmp16, mloc, mm_[:, 1:17], ALU.subtract, [B_sm], [B_sm])
                act(sloc, tmp16, AF.Exp, [B_sm], [B_sm])
                tok4, B_tok4 = NT([128, 4, 16, 4], F32, 'tok4')
                ssB, B_ssB = NT([128, 2, 16, 4], F32, 'ssB')
                R2F, B_R2 = NT([128, 2, 16, 4], F32, 'R2')
                memset('dve', R2F, 0.0, [B_R2])
                R2 = R2F[0:4]
                eye4 = identf[0:4, 0:4]

                def tok_tr(q, X, B_X):
                    for c in range(16):
                        mm(PS[5][:, q * 64 + c * 4:q * 64 + c * 4 + 4], X[:, c * 128:(c + 1) * 128], identf[:, 0:4], True, True, [B_X], [PSB[5]])
                tok_tr(0, G1, B_G1)
                M3 = G0[0:4, :].rearrange("p (c l) -> p c l", l=128)
                G33 = G3[0:4, :].rearrange("p (c l) -> p c l", l=128)
                tt('dve', G33, g3, gmax.unsqueeze(2).to_broadcast([4, 16, 128]), ALU.subtract, [B_G1, B_sm], [B_G3])
                act(G3[0:4, :], G3[0:4, :], AF.Exp, [B_G3], [B_G3])
                tok_tr(1, G3, B_G3)
                tt('dve', G33, mm_[:, 0:16].unsqueeze(2).to_broadcast([4, 16, 128]), M3, ALU.subtract, [B_sm, B_G0], [B_G3])
                act(G3[0:4, :], G3[0:4, :], AF.Exp, [B_G3], [B_G3])
                tok_tr(2, G3, B_G3)
                tt('dve', G3[0:4, :], G2[0:4, :], G0[0:4, :], ALU.add, [B_G2, B_G0], [B_G3])
                act(G3[0:4, :], G3[0:4, :], AF.Exp, [B_G3], [B_G3], scale=-1.0)
                tok_tr(3, G3, B_G3)
                cp('act', tok4.rearrange("p q c h -> p (q c h)"), PS[5][:, 0:256], [PSB[5]], [B_tok4])
                ts('dve', G2[0:4, :], G0[0:4, :], -1.0, ALU.mult, [B_G0], [B_G2])
                for i, sv in enumerate((sold, sloc)):
                    tt('dve', R2[:, i], sv.unsqueeze(2).to_broadcast([4, 16, 4]), eye4.unsqueeze(1).to_broadcast([4, 16, 4]),
                       ALU.mult, [B_sm, B_c], [B_R2])
                mm(PS[6][:, 0:128], onesf[:], R2F.rearrange("p i c h -> p (i c h)"), True, True, [B_R2], [PSB[6]])
                cp('act', ssB.rearrange("p i c h -> p (i c h)"), PS[6][:, 0:128], [PSB[6]], [B_ssB])
                S.barrier()
                ktok, B_ktok = NT([128, 16, 256], BF16, 'ktok')
                vx, B_vx = NT([128, 16, 258], BF16, 'vx')
                vpe, B_vpe = NT([128, 16, 258], BF16, 'vpe')
                ybuf, B_yb = NT([128, 2, 2048], BF16, 'ybuf')
                CTf, B_CTf = NT([128, 2, 258], F32, 'CTf')
                CTb, B_CTb = NT([128, 2, 258], BF16, 'CTb')
                PT, B_PT = SM[0][:, 0:128], SMB[0]
                qkT, B_qkT = NT([128, 128], BF16, 'qkT')
                tb, B_tb = SM[1][:, 0:257], SMB[1]
                tot, B_tot = SM[2][:, 0:257], SMB[2]
                hh, B_hh = SM[3][:, 0:256], SMB[3]
                st6, B_st6 = NT([128, 8], F32, 'st6')
                mv, B_mv = NT([128, 2], F32, 'mv')
                qsT, B_qsT = WS[0][:, 0:4096].rearrange("p (b t) -> p b t", t=2048), Buf('qsT')
                kT, B_kT = WS[1][:, 0:4096].rearrange("p (b t) -> p b t", t=2048), Buf('kT')
                vT, B_vT = WS[2][:, 0:4096].rearrange("p (b t) -> p b t", t=2048), Buf('vT')
                sigo = [(EV[i][:], EVB[i]) for i in range(2)]
                PSbf7 = PS[7][:].bitcast(BF16)
                memset('dve', vx[:, :, 256:258], 1.0, [B_vx])
                for h in range(4):
                    for (dst, B_dst, src) in ((qsT, B_qsT, mq_d), (kT, B_kT, mk_d), (vT, B_vT, mv_d)):
                        dma('sp', dst, src[h * 256:(h + 1) * 256, :].rearrange("(b p) t -> p b t", p=128), [B_mq], [B_dst])
                    for bl in range(2):
                        dma('sp', sigo[bl][0], moT[(2 * h + bl) * 128:(2 * h + bl + 1) * 128, :], [B_proj], [sigo[bl][1]])
                        act(sigo[bl][0], sigo[bl][0], AF.Sigmoid, [sigo[bl][1]], [sigo[bl][1]])
                    for bl in range(2):
                        for c8 in range(2):
                            for j in range(8):
                                c = c8 * 8 + j
                                tr(PSbf7[:, j * 128:(j + 1) * 128], kT[:, bl, c * 128:(c + 1) * 128], identb[:], [B_kT], [PSB[7]])
                            cp('act', ktok[:, c8 * 8:(c8 + 1) * 8, bl * 128:(bl + 1) * 128],
                               PSbf7[:, 0:1024].rearrange("p (c n) -> p c n", n=128), [PSB[7]], [B_ktok])
                            for j in range(8):
                                c = c8 * 8 + j
                                tr(PSbf7[:, j * 128:(j + 1) * 128], vT[:, bl, c * 128:(c + 1) * 128], identb[:], [B_vT], [PSB[7]])
                            pv = PSbf7[:, 0:1024].rearrange("p (c n) -> p c n", n=128)
                            cp('act', vx[:, c8 * 8:(c8 + 1) * 8, bl * 128:(bl + 1) * 128], pv, [PSB[7]], [B_vx])
                            tt('dve', vpe[:, c8 * 8:(c8 + 1) * 8, bl * 128:(bl + 1) * 128], pv,
                               tok4[:, 1, c8 * 8:(c8 + 1) * 8, h:h + 1].to_broadcast([128, 8, 128]), ALU.mult, [PSB[7], B_tok4], [B_vpe])
                    for j in range(2):
                        cp('dve', vpe[:, :, 256 + j], tok4[:, 1, :, h], [B_tok4], [B_vpe])
                    ts('dve', G3[0:4, :], G2[0:4, :], identf[0:4, h:h + 1], ALU.mult, [B_G2, B_c], [B_G3])
                    memset('dve', CTf, 0.0, [B_CTf])
                    memset('dve', CTb, 0.0, [B_CTb])
                    def front_a(c):
                        cs_ = slice(c * 128, (c + 1) * 128)
                        mm(PS[0][:, 0:128], kT[:, 0, cs_], qsT[:, 0, cs_], True, False, [B_kT, B_qsT], [PSB[0]])
                        mm(PS[0][:, 0:128], kT[:, 1, cs_], qsT[:, 1, cs_], False, True, [B_kT, B_qsT], [PSB[0]])
                        mm(PS[1][:, 0:128], identb[:], mneg[:, 0:128], True, False, [B_c], [PSB[1]])
                        mm(PS[1][:, 0:128], onesf[:], G3[:, cs_], False, True, [B_G3], [PSB[1]])
                        act(PT, PS[1][:, 0:128], AF.Exp, [PSB[1], B_tok4], [B_PT], bias=tok4[:, 0, c, h:h + 1])

                    def front_b(c):
                        cs_ = slice(c * 128, (c + 1) * 128)
                        tt('dve', qkT, PT, PS[0][:, 0:128], ALU.mult, [B_PT, PSB[0]], [B_qkT])
                        mm(PS[2][:, 0:257], qkT, vx[:, c, 0:257], True, True, [B_qkT, B_vx], [PSB[2]])
                        mm(PS[3][:, 0:257], qsT[:, 0, cs_], CTb[:, 0, 0:257], True, False, [B_qsT, B_CTb], [PSB[3]])
                        mm(PS[3][:, 0:257], qsT[:, 1, cs_], CTb[:, 1, 0:257], False, True, [B_qsT, B_CTb], [PSB[3]])

                    def mid(c):
                        ts('dve', tb, PS[3][:, 0:257], tok4[:, 2, c, h:h + 1], ALU.mult, [PSB[3], B_tok4], [B_tb])
                        tt('dve', tot, tb, PS[2][:, 0:257], ALU.add, [B_tb, PSB[2]], [B_tot])

                    def state(c):
                        for eb in range(2):
                            mm(PS[5 + eb][:, 0:257], ktok[:, c, eb * 128:(eb + 1) * 128], vpe[:, c, 0:257], True, True,
                               [B_ktok, B_vpe], [PSB[5 + eb]])
                        for eb in range(2):
                            ts('dve', CTf[:, eb, 0:257], CTf[:, eb, 0:257], ssB[:, 0, c, h:h + 1], ALU.mult, [B_CTf, B_ssB], [B_CTf])
                            stt(CTf[:, eb, 0:257], PS[5 + eb][:, 0:257], ssB[:, 1, c, h:h + 1], CTf[:, eb, 0:257], ALU.mult, ALU.add,
                                [PSB[5 + eb], B_ssB, B_CTf], [B_CTf])
                        cp('act', CTb[:, :, 0:257], CTf[:, :, 0:257], [B_CTf], [B_CTb])

                    def tail_1(c):
                        ts('dve', st6[:, 7:8], tot[:, 256:257], -1.0, ALU.mult, [B_tot], [B_st6])
                        tt('dve', st6[:, 6:7], tot[:, 256:257], st6[:, 7:8], ALU.max, [B_tot, B_st6], [B_st6])
                        tt('dve', st6[:, 6:7], st6[:, 6:7], tok4[:, 3, c, h:h + 1], ALU.max, [B_st6, B_tok4], [B_st6])
                        recip(st6[:, 6:7], st6[:, 6:7], [B_st6], [B_st6])
                        ts('dve', hh, tot[:, 0:256], st6[:, 6:7], ALU.mult, [B_tot, B_st6], [B_hh])
                        S.add('dve', lambda e: e.bn_stats(out=st6[:, 0:6], in_=hh), [B_hh], [B_st6])
                        S.add('dve', lambda e: e.bn_aggr(out=mv, in_=st6[:, 0:6]), [B_st6], [B_mv])
                        act(mv[:, 1:2], mv[:, 1:2], AF.Ln, [B_mv], [B_mv], bias=EPS)

                    def tail_2(c):
                        cs_ = slice(c * 128, (c + 1) * 128)
                        act(mv[:, 1:2], mv[:, 1:2], AF.Exp, [B_mv], [B_mv], scale=-0.5)
                        ts('dve', hh, hh, mv[:, 0:1], ALU.subtract, [B_hh, B_mv], [B_hh], s2=mv[:, 1:2], op1=ALU.mult)
                        for bl in range(2):
                            tr(PS[4][:, bl * 128:(bl + 1) * 128], hh[:, bl * 128:(bl + 1) * 128], identf[:], [B_hh], [PSB[4]])
                            col = CVM['mnw'] + 2 * h + bl
                            stt(ybuf[:, bl, cs_], PS[4][:, bl * 128:(bl + 1) * 128], cvt[:, col:col + 1], sigo[bl][0][:, cs_],
                                ALU.mult, ALU.mult, [PSB[4], sigo[bl][1]], [B_yb])

                    front_a(0)
                    front_b(0)
                    for c in range(16):
                        mid(c)
                        state(c)
                        if c + 1 < 16:
                            front_a(c + 1)
                        tail_1(c)
                        if c + 1 < 16:
                            front_b(c + 1)
                        tail_2(c)
                    dma('sp', yT[3072 + h * 256:3072 + (h + 1) * 256, :].rearrange("(b p) t -> p b t", p=128), ybuf, [B_yb], [B_y])

            if 'lru' in mixers:
                lru()
                S.barrier()
            if 'ssd' in mixers:
                ssd()
                S.barrier()
            if 'ml' in mixers:
                mlstm()
                S.barrier()

        if mixtest:
            mixer_stage(0)
            nl = 0
        else:
            stage0()
            S.barrier()
        for l in range(nl):
            uT = arv(0, [128, 16, 2048], BF16)
            B_u = Buf('uT')
            norm_stage(cvs[l], CVM['n1'], uT, B_u, 0, 2048)
            proj_stage(l, uT, B_u)
            S.barrier()
            mixer_stage(l)
            S.barrier()
            wout_stage(l)
            if do_ffn:
                ffn_stage(l)
        if not mixtest:
            final_stage()
        S.emit()
    return nc, S


def host_prep(inputs):
    f = lambda k: np.asarray(inputs[k], dtype=np.float32)
    cv = np.zeros((NL, 128, NCV), np.float32)

    def put(l, name, vec, nblk):
        cv[l, :, CVM[name]:CVM[name] + nblk] = vec.reshape(nblk, 128).T
    for l in range(NL):
        put(l, 'n1', f('norm1_w')[l], 16)
        put(l, 'n2', f('norm2_w')[l], 16)
        scw = f('ssd_conv_w')[l]
        for k in range(4):
            cv[l, :, CVM['scw'] + k * 32:CVM['scw'] + (k + 1) * 32] = scw[k].reshape(32, 128).T
        put(l, 'scb', f('ssd_conv_b')[l], 32)
        put(l, 'sd', np.repeat(f('ssd_d')[l], 64), 16)
        put(l, 'snw', f('ssd_norm_w')[l], 16)
        lcw = f('lru_conv_w')[l]
        for k in range(4):
            cv[l, :, CVM['lcw'] + k * 8:CVM['lcw'] + (k + 1) * 8] = lcw[k].reshape(8, 128).T
        put(l, 'lcb', f('lru_conv_b')[l], 8)
        put(l, 'lba', f('lru_b_a')[l], 8)
        put(l, 'lbx', f('lru_b_x')[l], 8)
        put(l, 'lam', f('lru_lambda')[l], 8)
        mcw = f('ml_conv_w')[l]
        for k in range(4):
            cv[l, :, CVM['mcw'] + k * 8:CVM['mcw'] + (k + 1) * 8] = mcw[k].reshape(8, 128).T
        put(l, 'mcb', f('ml_conv_b')[l], 8)
        put(l, 'mnw', f('ml_norm_w')[l], 8)
        put(l, 'nf', f('norm_f_w'), 16)
    hv = np.zeros((NL, 32, 4), np.float32)
    hv[:, :, 0] = f('ssd_dt_bias')
    hv[:, :, 1] = f('ssd_a_log')
    hv[:, 0:4, 2] = f('ml_b_if')[:, 0:4]
    hv[:, 0:4, 3] = f('ml_b_if')[:, 4:8]
    bdq = np.zeros((NL, 3, 8, 128, 128), np.float32)
    for i, nm in enumerate(['ml_w_q', 'ml_w_k', 'ml_w_v']):
        w = f(nm)
        for n in range(256):
            b, o = divmod(n, 32)
            bdq[:, i, b, o * 4:o * 4 + 4, o * 4:o * 4 + 4] = w[:, n]
    lbd = np.zeros((NL, 2, 8, 128, 128), np.float32)
    for i, nm in enumerate(['lru_w_a', 'lru_w_x']):
        w = f(nm)
        for n in range(16):
            b, o = divmod(n, 2)
            lbd[:, i, b, o * 64:o * 64 + 64, o * 64:o * 64 + 64] = w[:, n]
    wif = np.ascontiguousarray(f('ml_w_if').reshape(NL, 24, 128, 8).transpose(0, 2, 1, 3))
    cst = np.zeros((128, 640), np.float32)
    cst[:, 0:128] = np.eye(128, dtype=np.float32)
    s_ = np.arange(128)[:, None]
    t_ = np.arange(128)[None, :]
    mk = np.where(s_ <= t_, 0.0, NEG).astype(np.float32)
    cst[:, 128:640] = np.tile(mk, (1, 4))
    c32 = np.zeros((128, 2048 + 4 + 1024 + 2048), np.float32)
    rm = np.ones(2048, np.float32)
    rm[::128] = 0.0
    c32[0:32, 0:2048] = rm[None, :]
    hh = np.arange(32)
    for j in range(4):
        c32[0:32, 2048 + j] = (hh % 4 == j)
    for g in range(8):
        c32[0:32, 2052 + g * 128:2052 + (g + 1) * 128] = (hh // 4 == g)[:, None]
    for b in range(16):
        ch = np.arange(128)
        c32[0:32, 3076 + b * 128:3076 + (b + 1) * 128] = (hh[:, None] == (2 * b + ch[None, :] // 64))
    shared = {"w_in": f('w_in'), "w_out": f('w_out'), "w_gate_up": f('w_gate_up'), "w_down": f('w_down'),
              "cv": cv, "hv": hv, "bdq": bdq, "lbd": lbd, "wif": wif, "cst": cst, "c32": c32,
              "nfb": f('norm_f_w').reshape(1, D)}
    return shared


_CACHE = {}


def kernel(**inputs):
    shared = host_prep(inputs)
    if 'nc' not in _CACHE:
        _CACHE['nc'] = build()[0]
    nc = _CACHE['nc']
    xs = np.asarray(inputs['x'], dtype=np.float32)
    in_maps = []
    for b in range(8):
        m = dict(shared)
        m["x"] = np.ascontiguousarray(xs[b])
        in_maps.append(m)
    res = run_bass_kernel_spmd(nc, in_maps, core_ids=list(range(8)))
    return np.stack([np.asarray(r["out"]) for r in res.results], axis=0).astype(np.float32)
```
